# Optimizing a Trainium2 kernel written in Bass

```python
import math
import jax
import jax.numpy as jnp
from jax import lax
import numpy as np

D_MODEL = 1024
BATCH = 8
SEQ = 4096
DEPTH = 4

MIX_W = D_MODEL // 4
N_BRANCH = 4
CHUNK = 64
EPS = 1e-6
ROPE_BASE = 10000.0
CONV_K = 4

RET_HEADS = 4
RET_DK = MIX_W // RET_HEADS
RET_DV = MIX_W // RET_HEADS

SSD_HEADS = 4
SSD_P = MIX_W // SSD_HEADS
SSD_N = 128
SSD_GROUPS = 2
SSD_CONV_CH = MIX_W + 2 * SSD_GROUPS * SSD_N

GLA_HEADS = 4
GLA_DK = MIX_W // (2 * GLA_HEADS)
GLA_DV = MIX_W // GLA_HEADS
GLA_RANK = 16
GLA_TAU = 16.0

ML_HEADS = 4
ML_DK = MIX_W // ML_HEADS
ML_DV = MIX_W // ML_HEADS

D_FF = 2816
N_EXPERTS = 8
TOP_K = 2
EXPERT_FF = 2816

IN_SIZES = (
    MIX_W, MIX_W, MIX_W, MIX_W,
    MIX_W, SSD_CONV_CH, SSD_HEADS,
    GLA_HEADS * GLA_DK, GLA_HEADS * GLA_DK, MIX_W, MIX_W, GLA_RANK,
    2 * MIX_W, MIX_W, MIX_W, ML_HEADS, ML_HEADS,
    N_BRANCH * D_MODEL,
)
IN_COLS = sum(IN_SIZES)

kernel_name = 'hybrid_parallel_mixers_moe'


def rmsnorm(x, g):
    xf = x.astype(jnp.float32)
    y = xf * lax.rsqrt(jnp.mean(xf * xf, axis=-1, keepdims=True) + EPS)
    return (y * g.astype(jnp.float32)).astype(x.dtype)


def head_rmsnorm(t, g):
    nh, d = t.shape[-2], t.shape[-1]
    return rmsnorm(t, g.reshape(nh, d))


def rotary(t, pos):
    half = t.shape[-1] // 2
    inv = ROPE_BASE ** (-jnp.arange(half, dtype=jnp.float32) / half)
    ang = pos.astype(jnp.float32)[:, None] * inv[None, :]
    cos = jnp.cos(ang)[None, :, None, :]
    sin = jnp.sin(ang)[None, :, None, :]
    t1 = t[..., :half].astype(jnp.float32)
    t2 = t[..., half:].astype(jnp.float32)
    return jnp.concatenate([t1 * cos - t2 * sin, t1 * sin + t2 * cos], axis=-1)


def causal_dwconv(u, w, b):
    ch = u.shape[-1]
    y = lax.conv_general_dilated(u, w[:, None, :].astype(u.dtype), window_strides=(1,),
                                 padding=[(CONV_K - 1, 0)],
                                 dimension_numbers=('NWC', 'WIO', 'NWC'),
                                 feature_group_count=ch)
    return y + b


def _chunk(t):
    t = t.astype(jnp.float32)
    bsz, seq, nh = t.shape[:3]
    t = t.reshape((bsz, seq // CHUNK, CHUNK, nh) + t.shape[3:])
    return jnp.moveaxis(t, (1, 3), (0, 2))


def _unchunk(t):
    t = jnp.moveaxis(t, (0, 2), (1, 3))
    return t.reshape((t.shape[0], t.shape[1] * t.shape[2]) + t.shape[3:])


def _tri():
    return jnp.tril(jnp.ones((CHUNK, CHUNK), dtype=bool))


def retention_scan(q, k, v, log_gamma):
    bsz, _, nh, dk = q.shape
    dv = v.shape[-1]
    idx = jnp.arange(CHUNK, dtype=jnp.float32)
    rel = idx[:, None] - idx[None, :]
    tri = rel >= 0
    decay = jnp.where(tri[None], jnp.exp(jnp.where(tri, rel, 0.0)[None] * log_gamma[:, None, None]), 0.0)
    q_decay = jnp.exp((idx + 1.0)[None, :] * log_gamma[:, None])
    k_decay = jnp.exp((CHUNK - 1.0 - idx)[None, :] * log_gamma[:, None])
    chunk_decay = jnp.exp(CHUNK * log_gamma)

    def step(state, inp):
        qc, kc, vc = inp
        s = jnp.einsum('bhid,bhjd->bhij', qc, kc) * decay
        o = (jnp.einsum('bhij,bhjv->bhiv', s, vc)
             + jnp.einsum('bhid,bhdv->bhiv', qc, state) * q_decay[:, :, None])
        state = (state * chunk_decay[:, None, None]
                 + jnp.einsum('bhjd,bhjv->bhdv', kc * k_decay[:, :, None], vc))
        return state, o

    s0 = jnp.zeros((bsz, nh, dk, dv), jnp.float32)
    _, o = lax.scan(step, s0, (_chunk(q), _chunk(k), _chunk(v)))
    return _unchunk(o)


def ssd_scan(x, dt, bmat, cmat, a):
    bsz, _, nh, p = x.shape
    n = bmat.shape[-1]
    tri = _tri()

    def step(state, inp):
        xc, dtc, bc, cc = inp
        acs = jnp.cumsum(dtc * a[None, :, None], axis=-1)
        seg = jnp.exp(jnp.where(tri, acs[..., :, None] - acs[..., None, :], -jnp.inf))
        scores = jnp.einsum('bhin,bhjn->bhij', cc, bc) * seg
        xdt = xc * dtc[..., None]
        y = (jnp.einsum('bhij,bhjp->bhip', scores, xdt)
             + jnp.einsum('bhin,bhpn->bhip', cc, state) * jnp.exp(acs)[..., None])
        w_end = jnp.exp(acs[..., -1:] - acs)
        state = (state * jnp.exp(acs[..., -1])[..., None, None]
                 + jnp.einsum('bhjp,bhjn->bhpn', xdt * w_end[..., None], bc))
        return state, y

    s0 = jnp.zeros((bsz, nh, p, n), jnp.float32)
    _, y = lax.scan(step, s0, (_chunk(x), _chunk(dt), _chunk(bmat), _chunk(cmat)))
    return _unchunk(y)


def gla_scan(q, k, v, log_alpha):
    bsz, _, nh, dk = q.shape
    dv = v.shape[-1]
    tri = _tri()

    def step(state, inp):
        qc, kc, vc, gc = inp
        g = jnp.cumsum(gc, axis=2)
        seg = jnp.exp(jnp.where(tri[:, :, None], g[:, :, :, None, :] - g[:, :, None, :, :], -jnp.inf))
        att = jnp.sum(qc[:, :, :, None, :] * kc[:, :, None, :, :] * seg, axis=-1)
        o = (jnp.einsum('bhij,bhjv->bhiv', att, vc)
             + jnp.einsum('bhid,bhdv->bhiv', qc * jnp.exp(g), state))
        g_end = g[:, :, -1:, :]
        state = (state * jnp.exp(g_end[:, :, 0, :])[..., None]
                 + jnp.einsum('bhjd,bhjv->bhdv', kc * jnp.exp(g_end - g), vc))
        return state, o

    s0 = jnp.zeros((bsz, nh, dk, dv), jnp.float32)
    _, o = lax.scan(step, s0, (_chunk(q), _chunk(k), _chunk(v), _chunk(log_alpha)))
    return _unchunk(o)


def mlstm_scan(q, k, v, i_pre, log_f):
    bsz, _, nh, dk = q.shape
    dv = v.shape[-1]
    tri = _tri()

    def step(carry, inp):
        c_st, n_st, m = carry
        qc, kc, vc, ic, fc = inp
        b = jnp.cumsum(fc, axis=-1)
        dlog = jnp.where(tri, b[..., :, None] - b[..., None, :] + ic[..., None, :], -jnp.inf)
        inter = b + m[..., None]
        m_t = jnp.maximum(inter, jnp.max(dlog, axis=-1))
        s = jnp.einsum('bhid,bhjd->bhij', qc, kc) * jnp.exp(dlog - m_t[..., None])
        inter_w = jnp.exp(inter - m_t)
        num = (jnp.einsum('bhij,bhjv->bhiv', s, vc)
               + jnp.einsum('bhid,bhdv->bhiv', qc, c_st) * inter_w[..., None])
        den = jnp.sum(s, axis=-1) + jnp.einsum('bhid,bhd->bhi', qc, n_st) * inter_w
        h = num / jnp.maximum(jnp.abs(den), jnp.exp(-m_t))[..., None]
        b_end = b[..., -1]
        w_log = b_end[..., None] - b + ic
        m_new = jnp.maximum(b_end + m, jnp.max(w_log, axis=-1))
        w = jnp.exp(w_log - m_new[..., None])
        carry_decay = jnp.exp(b_end + m - m_new)
        kw = kc * w[..., None]
        c_st = c_st * carry_decay[..., None, None] + jnp.einsum('bhjd,bhjv->bhdv', kw, vc)
        n_st = n_st * carry_decay[..., None] + jnp.sum(kw, axis=2)
        return (c_st, n_st, m_new), h

    init = (jnp.zeros((bsz, nh, dk, dv), jnp.float32),
            jnp.zeros((bsz, nh, dk), jnp.float32),
            jnp.zeros((bsz, nh), jnp.float32))
    _, h = lax.scan(step, init, (_chunk(q), _chunk(k), _chunk(v), _chunk(i_pre), _chunk(log_f)))
    return _unchunk(h)


def _split_cols(u):
    outs, start = [], 0
    for width in IN_SIZES:
        outs.append(u[..., start:start + width])
        start += width
    return outs


def token_mixers(h, pos, w_in, ret_norm_g, ssd_conv_w, ssd_conv_b, ssd_dt_bias, ssd_a_log, ssd_d,
                 ssd_norm_g, gla_w_alpha, gla_b_alpha, gla_norm_g, ml_conv_w, ml_conv_b, ml_b_i,
                 ml_b_f, ml_norm_g, w_branch, w_out):
    bsz, seq, _ = h.shape
    u = h @ w_in
    (rq, rk, rv, rg, sz, sxbc, sdt, gq, gk, gv, gr, ga,
     mqk, mv, mo, mi, mf, mgate) = _split_cols(u)

    def heads(t, nh):
        return t.reshape(bsz, seq, nh, -1)

    log_gamma = jnp.log1p(-jnp.exp2(-5.0 - jnp.arange(RET_HEADS, dtype=jnp.float32)))
    q = rotary(heads(rq, RET_HEADS), pos)
    k = rotary(heads(rk, RET_HEADS), pos) * (RET_DK ** -0.5)
    o = retention_scan(q, k, heads(rv, RET_HEADS), log_gamma)
    y_ret = (head_rmsnorm(o, ret_norm_g).reshape(bsz, seq, MIX_W) * jax.nn.silu(rg)).astype(h.dtype)

    xbc = jax.nn.silu(causal_dwconv(sxbc, ssd_conv_w, ssd_conv_b))
    sx = xbc[..., :MIX_W]
    sb = xbc[..., MIX_W:MIX_W + SSD_GROUPS * SSD_N].reshape(bsz, seq, SSD_GROUPS, SSD_N)
    sc = xbc[..., MIX_W + SSD_GROUPS * SSD_N:].reshape(bsz, seq, SSD_GROUPS, SSD_N)
    rep = SSD_HEADS // SSD_GROUPS
    bh = jnp.repeat(sb, rep, axis=2)
    ch = jnp.repeat(sc, rep, axis=2)
    dt = jax.nn.softplus(sdt.astype(jnp.float32) + ssd_dt_bias.astype(jnp.float32))
    a = -jnp.exp(ssd_a_log.astype(jnp.float32))
    xs = heads(sx, SSD_HEADS).astype(jnp.float32)
    y = ssd_scan(xs, dt, bh, ch, a) + ssd_d.astype(jnp.float32)[:, None] * xs
    y_ssd = rmsnorm(y.reshape(bsz, seq, MIX_W) * jax.nn.silu(sz), ssd_norm_g).astype(h.dtype)

    alpha_pre = (ga @ gla_w_alpha + gla_b_alpha).astype(jnp.float32)
    log_alpha = heads(jax.nn.log_sigmoid(alpha_pre) / GLA_TAU, GLA_HEADS)
    q = heads(gq, GLA_HEADS) * (GLA_DK ** -0.5)
    o = gla_scan(q, heads(gk, GLA_HEADS), heads(gv, GLA_HEADS), log_alpha)
    y_gla = (head_rmsnorm(o, gla_norm_g).reshape(bsz, seq, MIX_W) * jax.nn.silu(gr)).astype(h.dtype)

    qk = jax.nn.silu(causal_dwconv(mqk, ml_conv_w, ml_conv_b))
    q = heads(qk[..., :MIX_W], ML_HEADS)
    k = heads(qk[..., MIX_W:], ML_HEADS) * (ML_DK ** -0.5)
    i_pre = mi.astype(jnp.float32) + ml_b_i.astype(jnp.float32)
    log_f = jax.nn.log_sigmoid(mf.astype(jnp.float32) + ml_b_f.astype(jnp.float32))
    o = mlstm_scan(q, k, heads(mv, ML_HEADS), i_pre, log_f)
    y_ml = (head_rmsnorm(o, ml_norm_g).reshape(bsz, seq, MIX_W) * jax.nn.sigmoid(mo)).astype(h.dtype)

    ybr = jnp.stack([y_ret, y_ssd, y_gla, y_ml], axis=2)
    gates = jax.nn.sigmoid(mgate.reshape(bsz, seq, N_BRANCH, D_MODEL))
    merged = jnp.sum(gates * jnp.einsum('bsnv,nvd->bsnd', ybr, w_branch), axis=2)
    return merged @ w_out


def swiglu(h, w1, w3, w2):
    return (jax.nn.silu(h @ w1) * (h @ w3)) @ w2


def moe_ffn(h, router_w, w1, w3, w2):
    logits = (h @ router_w).astype(jnp.float32)
    top_vals, top_idx = lax.top_k(logits, TOP_K)
    top_w = jax.nn.softmax(top_vals, axis=-1)
    gate = jnp.sum(jax.nn.one_hot(top_idx, N_EXPERTS, dtype=jnp.float32) * top_w[..., None], axis=-2)
    out = jnp.zeros_like(h)
    for e in range(N_EXPERTS):
        out = out + gate[..., e:e + 1].astype(h.dtype) * swiglu(h, w1[e], w3[e], w2[e])
    return out


def setup_inputs(seed: int = 0) -> dict:
    key = jax.random.key(seed)
    ks = jax.random.split(key, 40)
    f32 = jnp.float32

    def nrm(i, shape, scale):
        return scale * jax.random.normal(ks[i], shape, f32)

    def gain(i, shape):
        return 1.0 + nrm(i, shape, 0.02)

    nl = DEPTH
    nd = (DEPTH + 1) // 2
    nm = DEPTH // 2
    res = (2.0 * DEPTH) ** -0.5
    dt = jnp.exp(jax.random.uniform(ks[8], (nl, SSD_HEADS), f32, math.log(1e-3), math.log(1e-1)))
    return {
        'x': nrm(0, (BATCH, SEQ, D_MODEL), 1.0),
        'mix_norm_g': gain(1, (nl, D_MODEL)),
        'w_in': nrm(2, (nl, D_MODEL, IN_COLS), D_MODEL ** -0.5),
        'ret_norm_g': gain(3, (nl, MIX_W)),
        'ssd_conv_w': nrm(4, (nl, CONV_K, SSD_CONV_CH), CONV_K ** -0.5),
        'ssd_conv_b': nrm(5, (nl, SSD_CONV_CH), 0.02),
        'ssd_dt_bias': dt + jnp.log(-jnp.expm1(-dt)),
        'ssd_a_log': jnp.log(jax.random.uniform(ks[9], (nl, SSD_HEADS), f32, 1.0, 16.0)),
        'ssd_d': 1.0 + nrm(10, (nl, SSD_HEADS), 0.1),
        'ssd_norm_g': gain(11, (nl, MIX_W)),
        'gla_w_alpha': nrm(12, (nl, GLA_RANK, GLA_HEADS * GLA_DK), GLA_RANK ** -0.5),
        'gla_b_alpha': nrm(13, (nl, GLA_HEADS * GLA_DK), 0.1),
        'gla_norm_g': gain(14, (nl, MIX_W)),
        'ml_conv_w': nrm(15, (nl, CONV_K, 2 * MIX_W), CONV_K ** -0.5),
        'ml_conv_b': nrm(16, (nl, 2 * MIX_W), 0.02),
        'ml_b_i': nrm(17, (nl, ML_HEADS), 0.1),
        'ml_b_f': jnp.linspace(3.0, 6.0, ML_HEADS, dtype=f32)[None, :] + nrm(18, (nl, ML_HEADS), 0.1),
        'ml_norm_g': gain(19, (nl, MIX_W)),
        'w_branch': nrm(20, (nl, N_BRANCH, MIX_W, D_MODEL), MIX_W ** -0.5),
        'w_out': nrm(21, (nl, D_MODEL, D_MODEL), res * D_MODEL ** -0.5),
        'ffn_norm_g': gain(22, (nl, D_MODEL)),
        'ffn_w1': nrm(23, (nd, D_MODEL, D_FF), D_MODEL ** -0.5),
        'ffn_w3': nrm(24, (nd, D_MODEL, D_FF), D_MODEL ** -0.5),
        'ffn_w2': nrm(25, (nd, D_FF, D_MODEL), res * D_FF ** -0.5),
        'router_w': nrm(26, (nm, D_MODEL, N_EXPERTS), D_MODEL ** -0.5),
        'moe_w1': nrm(27, (nm, N_EXPERTS, D_MODEL, EXPERT_FF), D_MODEL ** -0.5),
        'moe_w3': nrm(28, (nm, N_EXPERTS, D_MODEL, EXPERT_FF), D_MODEL ** -0.5),
        'moe_w2': nrm(29, (nm, N_EXPERTS, EXPERT_FF, D_MODEL), res * EXPERT_FF ** -0.5),
        'final_norm_g': gain(30, (D_MODEL,)),
    }


def reference(x, mix_norm_g, w_in, ret_norm_g, ssd_conv_w, ssd_conv_b, ssd_dt_bias, ssd_a_log, ssd_d,
              ssd_norm_g, gla_w_alpha, gla_b_alpha, gla_norm_g, ml_conv_w, ml_conv_b, ml_b_i, ml_b_f,
              ml_norm_g, w_branch, w_out, ffn_norm_g, ffn_w1, ffn_w3, ffn_w2, router_w, moe_w1, moe_w3,
              moe_w2, final_norm_g):
    pos = jnp.arange(x.shape[1], dtype=jnp.int32)
    for l in range(DEPTH):
        h = rmsnorm(x, mix_norm_g[l])
        x = x + token_mixers(h, pos, w_in[l], ret_norm_g[l], ssd_conv_w[l], ssd_conv_b[l], ssd_dt_bias[l],
                             ssd_a_log[l], ssd_d[l], ssd_norm_g[l], gla_w_alpha[l], gla_b_alpha[l],
                             gla_norm_g[l], ml_conv_w[l], ml_conv_b[l], ml_b_i[l], ml_b_f[l], ml_norm_g[l],
                             w_branch[l], w_out[l]).astype(x.dtype)
        h = rmsnorm(x, ffn_norm_g[l])
        if l % 2 == 0:
            x = x + swiglu(h, ffn_w1[l // 2], ffn_w3[l // 2], ffn_w2[l // 2]).astype(x.dtype)
        else:
            x = x + moe_ffn(h, router_w[l // 2], moe_w1[l // 2], moe_w3[l // 2], moe_w2[l // 2]).astype(x.dtype)
    return rmsnorm(x, final_norm_g)
```

```python
import math
from contextlib import ExitStack

import numpy as np
import ml_dtypes

import concourse.bass as bass
import concourse.mybir as mybir
from concourse.bass_utils import run_bass_kernel_spmd

F32 = mybir.dt.float32
BF16 = mybir.dt.bfloat16
AF = mybir.ActivationFunctionType
ALU = mybir.AluOpType
AX = mybir.AxisListType

D = 1024
NCOL = 7964
DFF = 2816
NE = 8
EPS = 1e-6
NDS = 40

FMW = 18 * 128 + 16
TMO = FMW
NA = FMW + 2048 + 12
RR_MIXG = 0
RR_FFNG = 1024
RR_RETG = 2048
RR_SSDG = 2304
RR_GLAG = 2560
RR_MLG = 2816
RR_DVEC = 3072
RR_DTB = 3328
RR_ALOG = 3332
RR_BI = 3336
RR_BF = 3340
NRR = 3344
CP_CW = 0
CP_CB = 40
NCP = 50


class Tk:
    __slots__ = ("w", "r")

    def __init__(self):
        self.w = None
        self.r = {}


class Buf:
    def __init__(self, t, excl=False):
        self.t = t
        self.k = Tk()
        self.excl = excl

    def __getitem__(self, idx):
        return self.t[idx]


class Prog:
    ENG = ("pe", "act", "dve", "pool", "sp")

    def __init__(self, nc, stack):
        self.nc = nc
        self.ops = {e: [] for e in self.ENG}
        self.sems = []
        self.esem = {}
        for e in ("pe", "act", "dve", "pool"):
            self.esem[e] = len(self.sems)
            self.sems.append(stack.enter_context(nc.semaphore("s_" + e)))
        self.cur = [0] * 4
        self.dsem = []
        for i in range(NDS):
            self.dsem.append(len(self.sems))
            self.sems.append(stack.enter_context(nc.semaphore("d%d" % i)))
            self.cur.append(0)
        self.dnext = {"sp": 0, "pool": 0}
        self.drange = {"sp": (0, 24), "pool": (24, NDS)}
        self.seen = {e: {} for e in self.ENG}

    def _waits(self, eng, reads, writes, extra=()):
        need = {}
        for t in reads:
            if t.w is not None:
                k, v = t.w
                if need.get(k, 0) < v:
                    need[k] = v
        for t in writes:
            if t.w is not None:
                k, v = t.w
                if need.get(k, 0) < v:
                    need[k] = v
            for k, v in t.r.items():
                if need.get(k, 0) < v:
                    need[k] = v
        for k, v in extra:
            if need.get(k, 0) < v:
                need[k] = v
        seen = self.seen[eng]
        out = []
        pe_own = self.esem["pe"]
        for k, v in need.items():
            if eng == "pe" and k == pe_own:
                continue
            if seen.get(k, 0) >= v:
                continue
            seen[k] = v
            out.append((k, v))
        return out

    def _track(self, ev, reads, writes):
        k, v = ev
        for t in reads:
            if t.r.get(k, 0) < v:
                t.r[k] = v
        for t in writes:
            t.w = ev
            t.r = {}

    region_on = False
    region_count = 0
    region_limit = 10 ** 9

    def op(self, eng, fn, reads=(), writes=()):
        if self.region_on:
            self.region_count += 1
            if self.region_count > self.region_limit:
                return
        writes = list(writes) + [b for b in reads if b.excl and b not in writes]
        reads = [b.k for b in reads]
        writes = [b.k for b in writes]
        waits = self._waits(eng, reads, writes)
        k = self.esem[eng]
        self.cur[k] += 1
        ev = (k, self.cur[k])
        self.ops[eng].append((waits, fn, k, 1))
        self._track(ev, reads, writes)

    def dma(self, q, out, in_, reads=(), writes=()):
        reads = [b.k for b in reads]
        writes = [b.k for b in writes]
        lo, hi = self.drange[q]
        j = lo + self.dnext[q]
        self.dnext[q] = (self.dnext[q] + 1) % (hi - lo)
        k = self.dsem[j]
        extra = [(k, self.cur[k])] if self.cur[k] > 0 else []
        waits = self._waits(q, reads, writes, extra)
        self.cur[k] += 16
        ev = (k, self.cur[k])
        self.ops[q].append((waits, (lambda e: e.dma_start(out=out, in_=in_)), k, 16))
        self._track(ev, reads, writes)

    def barrier(self, engs=None):
        for eng in (engs or self.ENG):
            seen = self.seen[eng]
            waits = []
            for k, v in enumerate(self.cur):
                if v > 0 and seen.get(k, 0) < v and not (eng == "pe" and k == self.esem["pe"]):
                    seen[k] = v
                    waits.append((k, v))
            self.ops[eng].append((waits, None, None, 0))

    def emit(self):
        nc = self.nc
        sems = self.sems

        def run(name, e):
            for waits, fn, sk, inc in self.ops[name]:
                for k, v in waits:
                    e.wait_ge(sems[k], v)
                if fn is None:
                    continue
                fn(e).then_inc(sems[sk], inc)

        with nc.Block() as block:
            @block.tensor
            def _(e):
                run("pe", e)

            @block.scalar
            def _(e):
                run("act", e)

            @block.vector
            def _(e):
                run("dve", e)

            @block.gpsimd
            def _(e):
                run("pool", e)

            @block.sync
            def _(e):
                run("sp", e)


def bc(ap, shape, axis):
    return ap.unsqueeze(axis).broadcast_to(list(shape))


class Bld:
    def __init__(self, nc, S, depth):
        self.nc = nc
        self.S = S
        self.depth = depth
        self.NT = S // 512
        import os
        self.en = set(os.environ.get('KSEC', 'ret,ssd,gla,ml').split(','))
        self.ph = os.environ.get('KPH', 'ABCD')

    def mm(self, out, lhsT, rhs, start, stop, rd, wr):
        self.P.op("pe", lambda e: e.matmul(out, lhsT=lhsT, rhs=rhs, start=start, stop=stop), rd, wr)

    def tr(self, out, in_, rd, wr):
        ident = self.ident
        self.P.op("pe", lambda e: e.transpose(out=out, in_=in_, identity=ident[:]), list(rd) + [ident], wr)

    def act(self, out, in_, func, rd, wr, scale=1.0, bias=None, accum=None):
        kw = {}
        if bias is not None:
            kw["bias"] = bias
        if accum is not None:
            kw["accum_out"] = accum
        self.P.op("act", lambda e: e.activation(out=out, in_=in_, func=func, scale=scale, **kw), rd, wr)

    def tt(self, eng, out, a, b, op, rd, wr):
        self.P.op(eng, lambda e: e.tensor_tensor(out=out, in0=a, in1=b, op=op), rd, wr)

    def ts(self, eng, out, a, s1, s2, op0, op1, rd, wr):
        if s2 is None:
            self.P.op(eng, lambda e: e.tensor_scalar(out=out, in0=a, scalar1=s1, scalar2=None, op0=op0), rd, wr)
        else:
            self.P.op(eng, lambda e: e.tensor_scalar(out=out, in0=a, scalar1=s1, scalar2=s2, op0=op0, op1=op1), rd, wr)

    def stt(self, eng, out, a, sc, b, op0, op1, rd, wr):
        self.P.op(eng, lambda e: e.scalar_tensor_tensor(out=out, in0=a, scalar=sc, in1=b, op0=op0, op1=op1), rd, wr)

    def cp(self, eng, out, in_, rd, wr):
        if eng == "act":
            self.P.op("act", lambda e: e.copy(out=out, in_=in_), rd, wr)
        else:
            self.P.op(eng, lambda e: e.tensor_copy(out=out, in_=in_), rd, wr)

    def ms(self, eng, ap, val, wr):
        self.P.op(eng, lambda e: e.memset(ap, val), [], wr)

    def psf(self):
        return self.pf_free.pop(0)

    def psb(self):
        return self.pb_free.pop(0)

    def prel(self, *bufs):
        for b in bufs:
            if b in self.pf:
                assert b not in self.pf_free
                self.pf_free.append(b)
            else:
                assert b not in self.pb_free
                self.pb_free.append(b)

    def wf(self, kind, n=1):
        pool = self.pf_free if kind == 'f' else self.pb_free
        while len(pool) < n:
            yield

    def sb(self, st, name, shape, dt):
        self.nid = getattr(self, "nid", 0) + 1
        return Buf(st.enter_context(self.nc.sbuf_tensor("%s_%d" % (name, self.nid), list(shape), dt)))

    def build(self):
        nc = self.nc
        S, L = self.S, self.depth
        ND = (L + 1) // 2
        NM = L // 2
        dr = lambda name, shape, dt=F32, kind="ExternalInput": nc.dram_tensor(name, list(shape), dt, kind=kind).ap()
        self.x_in = dr("x", [S, D])
        self.wA = dr("wA", [L, D, NA])
        self.w_in = dr("w_in", [L, D, NCOL])
        self.wbr = dr("w_branch", [L, 4, 256, D])
        self.wout = dr("w_out", [L, D, D])
        self.rowrep = dr("rowrep", [L, 128, NRR])
        self.colpack = dr("colpack", [L, 128, NCP])
        self.walaug = dr("walaug", [L, 33, 128])
        self.fw1 = dr("ffn_w1", [ND, D, DFF])
        self.fw3 = dr("ffn_w3", [ND, D, DFF])
        self.fw2 = dr("ffn_w2", [ND, DFF, D])
        self.rw = dr("router_w", [max(NM, 1), D, NE])
        self.mw1 = dr("moe_w1", [max(NM, 1), NE, D, DFF])
        self.mw3 = dr("moe_w3", [max(NM, 1), NE, D, DFF])
        self.mw2 = dr("moe_w2", [max(NM, 1), NE, DFF, D])
        self.fing = dr("fing", [128, D])
        self.c32 = dr("c32", [128, 5 * 128 + 8 + 256])
        self.identd = dr("identd", [128, 128], BF16)
        self.cosd = dr("cosT", [128, S])
        self.sind = dr("sinT", [128, S])
        self.out = dr("out", [S, D], F32, "ExternalOutput")
        self.xres = dr("xres", [S, D], F32, "Internal")
        self.hT_d = dr("hT_d", [8, 128, S], BF16, "Internal")
        self.yT_d = dr("yT_d", [8, 128, S], BF16, "Internal")
        self.hT2_d = dr("hT2_d", [8, 128, S], BF16, "Internal")

        with ExitStack() as st0:
            self.P = Prog(nc, st0)
            P = self.P
            self.pf = [Buf(st0.enter_context(nc.psum_tensor("pf%d" % i, [128, 512], F32)), True) for i in range(6)]
            self.pb = [Buf(st0.enter_context(nc.psum_tensor("pb%d" % i, [128, 1024], BF16)), True) for i in range(2)]
            self.pf_free = list(self.pf)
            self.pb_free = list(self.pb)
            self.ident = self.sb(st0, "ident", [128, 128], BF16)
            self.cc = self.sb(st0, "cc", [128, 5 * 128 + 8 + 256], F32)
            P.dma("sp", self.ident[:], self.identd, writes=[self.ident])
            P.dma("sp", self.cc[:], self.c32, writes=[self.cc])
            cc = self.cc
            self.tri = cc[:, 0:128]
            self.neg = cc[:, 128:256]
            self.ones = cc[:, 256:384]
            self.bd01 = cc[:, 384:512]
            self.kapR = cc[:, 640:644]
            self.qdec = cc[:, 644:648]
            self.EMr = cc[:, 648:904]
            self.gate = self.sb(st0, "gate", [128, S // 128, NE], F32)

            for l in range(L):
                xsrc = self.x_in if l == 0 else self.xres
                if 'A' in self.ph:
                    self.phaseA(l, xsrc)
                    P.barrier()
                if 'B' in self.ph:
                    self.phaseB(l, xsrc)
                    P.barrier()
                if 'C' in self.ph:
                    self.phaseC(l)
                    P.barrier()
            if 'D' in self.ph:
                self.phaseD()
            P.barrier()
            P.emit()
        return nc

    def phaseA(self, l, xsrc):
        nc, P, S, NT = self.nc, self.P, self.S, self.NT
        tri, neg, ones, bd01 = self.tri, self.neg, self.ones, self.bd01
        cc = self.cc
        with ExitStack() as st:
            sb = lambda name, shape, dt=F32: self.sb(st, name, shape, dt)
            wA = [sb("wA%d" % k, [128, NA], BF16) for k in range(8)]
            for k in range(8):
                P.dma("pool", wA[k][:], self.wA[l, k * 128:(k + 1) * 128, :], writes=[wA[k]])
            rr = sb("rr", [128, NRR])
            P.dma("sp", rr[:], self.rowrep[l], writes=[rr])
            cpk = sb("cpk", [128, NCP])
            P.dma("sp", cpk[:], self.colpack[l], writes=[cpk])
            wal = sb("wal", [33, 128], BF16)
            P.dma("pool", wal[:], self.walaug[l], writes=[wal])
            arep = sb("arep", [128, 4])
            self.act(arep[:], rr[:, RR_ALOG:RR_ALOG + 4], AF.Exp, [rr], [arep])
            self.ts("dve", arep[:], arep[:], -1.0, None, ALU.mult, None, [arep], [arep])

            xb = [sb("xb%d" % i, [128, D]) for i in range(2)]
            hb = sb("hb", [128, D], BF16)
            hT = sb("hT", [128, 8, 512], BF16)
            cosb = sb("cosb", [128, 512])
            sinb = sb("sinb", [128, 512])
            rtmp = [sb("rtmp%d" % i, [128, 512]) for i in range(2)]
            rqT = sb("rqT", [128, 2, 512], BF16)
            rkT = sb("rkT", [128, 2, 512], BF16)
            raw = [sb("raw%d" % i, [128, 515]) for i in range(2)]
            halo = sb("halo", [128, 10, 3])
            cacc = [sb("cacc%d" % i, [128, 512]) for i in range(2)]
            cvT = sb("cvT", [128, 10, 512], BF16)
            gaT = sb("gaT", [33, 512], BF16)
            st4 = sb("st4", [128, 12])
            smN = sb("smN", [128, 8])
            smR = sb("smR", [128, 16])
            smS = sb("smS", [128, 64])
            smG = sb("smG", [128, 16])
            smM = sb("smM", [128, 64])
            vr = sb("vr", [128, 4, 64], BF16)
            sgr = sb("sgr", [128, 256])
            e1 = sb("e1", [128, 128])
            lap = sb("lap", [128, 4, 64])
            eq = sb("eq", [128, 4, 64])
            ek = sb("ek", [128, 4, 64])
            qpad = sb("qpad", [128, 4, 64], BF16)
            kpad = sb("kpad", [128, 4, 64], BF16)
            qkTg = sb("qkTg", [128, 4, 128], BF16)
            vg = sb("vg", [128, 4, 64], BF16)
            vm = sb("vm", [128, 4, 65], BF16)
            sgm = sb("sgm", [128, 256])
            sgz = sb("sgz", [128, 512])
            ktr = sb("ktr", [128, 256], BF16)
            ktm = sb("ktm", [128, 256], BF16)
            xBt = sb("xBt", [128, 4, 128], BF16)
            Rb = sb("Rb", [128, 4, 128])
            Dm = sb("Dm", [128, 4, 128])
            pTs_ = {m: sb("pT" + m, [128, 4, 128], BF16) for m in "RSGM"}
            vs = sb("vs", [128, 4, 64], BF16)
            vs2 = sb("vs2", [128, 4, 64], BF16)
            ob = {m: [sb("o%s%d" % (m, i), [128, 256]) for i in range(3)] for m in "RSGM"}
            gsgs = {m: sb("gsg" + m, [128, 256]) for m in "RGM"}
            ytok = {m: sb("ytok" + m, [128, 256], BF16) for m in "RSGM"}
            yT = sb("yT", [128, 8, 512], BF16)
            Sr = sb("Sr", [128, 2, 128]); Sr16 = sb("Sr16", [128, 2, 128], BF16)
            Sg = sb("Sg", [128, 2, 128]); Sg16 = sb("Sg16", [128, 2, 128], BF16)
            Sm = sb("Sm", [128, 2, 130]); Sm16 = sb("Sm16", [128, 2, 130], BF16)
            Ss = sb("Ss", [128, 4, 64]); Ss16 = sb("Ss16", [128, 4, 64], BF16)
            EMg = sb("EMg", [128, 2, 128])
            EMm = sb("EMm", [128, 2, 130])
            for b_ in (Sr, Sg, Sm, Ss, EMm, lap, halo):
                self.ms("dve", b_[:], 0.0, [b_])
            for b_ in (Sr16, Sg16, Sm16, Ss16, qpad, kpad, gaT):
                self.ms("pool", b_[:], 0.0, [b_])
            self.ms("pool", gaT[32:33, :], 1.0, [gaT])

            def rstd(out, in_, scale, buf):
                self.act(out, in_, AF.Ln, [buf], [buf], scale=scale, bias=EPS)
                self.act(out, out, AF.Exp, [buf], [buf], scale=-0.5)

            def sigm(out, x_ap, xb_, ob_):
                self.act(out, x_ap, AF.Exp, [xb_], [ob_], scale=-1.0)
                self.act(out, out, AF.Ln, [ob_], [ob_], bias=1.0)
                self.act(out, out, AF.Exp, [ob_], [ob_], scale=-1.0)

            v4 = lambda ap: ap.rearrange("p (h v) -> p h v", h=4)

            for t in range(NT):
                tsl = slice(t * 512, (t + 1) * 512)
                for c in range(4):
                    row0 = (t * 4 + c) * 128
                    xt = xb[c % 2]
                    P.dma("sp", xt[:], xsrc[row0:row0 + 128, :], writes=[xt])
                    self.act(hb[:], xt[:], AF.Square, [xt], [hb, smN], accum=smN[:, 0:1])
                    rstd(smN[:, 1:2], smN[:, 0:1], 1.0 / D, smN)
                    self.stt("dve", hb[:], xt[:], smN[:, 1:2], rr[:, RR_MIXG:RR_MIXG + D], ALU.mult, ALU.mult, [xt, smN, rr], [hb])
                    pt = self.psb()
                    for k in range(8):
                        self.tr(pt[:, k * 128:(k + 1) * 128], hb[:, k * 128:(k + 1) * 128], [hb], [pt])
                    self.cp("act", hT[:, :, c * 128:(c + 1) * 128], pt[:].rearrange("p (k n) -> p k n", k=8), [pt], [hT])
                    self.prel(pt)
                P.dma("sp", self.hT_d[:, :, tsl].rearrange("k p n -> p k n"), hT[:], reads=[hT])
                P.dma("sp", cosb[:], self.cosd[:, tsl], writes=[cosb])
                P.dma("sp", sinb[:], self.sind[:, tsl], writes=[sinb])

                def fm(b, M=128):
                    ps = self.psf()
                    for k in range(8):
                        self.mm(ps[0:M, :], wA[k][:, b * 128:b * 128 + M], hT[:, k, :], k == 0, k == 7, [wA[k], hT], [ps])
                    return ps
                for qk, dst in ((0, rqT), (1, rkT)):
                    for p in range(2):
                        ps1 = fm(qk * 4 + p)
                        self.tt("dve", rtmp[0][:], ps1[:], cosb[:], ALU.mult, [ps1, cosb], [rtmp[0]])
                        self.prel(ps1)
                        ps2 = fm(qk * 4 + 2 + p)
                        self.tt("dve", rtmp[1][:], ps2[:], sinb[:], ALU.mult, [ps2, sinb], [rtmp[1]])
                        self.prel(ps2)
                        self.tt("pool", dst[:, p, :], rtmp[0][:], rtmp[1][:], ALU.add, [rtmp[0], rtmp[1]], [dst])
                ps = fm(18, M=16)
                self.cp("act", gaT[0:16, :], ps[0:16, :], [ps], [gaT])
                self.prel(ps)
                for b in range(10):
                    ps = fm(8 + b)
                    rw_ = raw[b % 2]
                    ac = cacc[b % 2]
                    self.cp("act", rw_[:, 3:515], ps[:], [ps], [rw_])
                    self.prel(ps)
                    self.cp("pool", rw_[:, 0:3], halo[:, b, :], [halo], [rw_])
                    cw = lambda k_: cpk[:, CP_CW + b * 4 + k_:CP_CW + b * 4 + k_ + 1]
                    self.ts("dve", ac[:], rw_[:, 0:512], cw(0), cpk[:, CP_CB + b:CP_CB + b + 1], ALU.mult, ALU.add, [rw_, cpk], [ac])
                    for k_ in range(1, 4):
                        self.stt("dve", ac[:], rw_[:, k_:k_ + 512], cw(k_), ac[:], ALU.mult, ALU.add, [rw_, cpk, ac], [ac])
                    self.cp("pool", halo[:, b, :], rw_[:, 512:515], [rw_], [halo])
                    self.act(cvT[:, b, :], ac[:], AF.Silu, [ac], [cvT])

                for c in range(4):
                    csl = slice(c * 128, (c + 1) * 128)

                    def tmproj(j, ncol):
                        ps = self.psf()
                        off = TMO + j * 512
                        for k in range(8):
                            self.mm(ps[:, 0:ncol], hT[:, k, csl], wA[k][:, off:off + ncol], k == 0, k == 7, [hT, wA[k]], [ps])
                        return ps

                    ps = tmproj(4, 12)
                    self.cp("act", st4[:], ps[:, 0:12], [ps], [st4])
                    self.prel(ps)
                    ps3 = tmproj(3, 512)
                    sigm(sgz[:], ps3[:], ps3, sgz)
                    self.tt("dve", sgz[:], sgz[:], ps3[:], ALU.mult, [sgz, ps3], [sgz])
                    self.prel(ps3)

                    def sec_ret():
                        sm = smR
                        o1, o2, o3 = ob["R"]
                        gsg = gsgs["R"]
                        yield from self.wf('f')
                        ps = tmproj(0, 512)
                        self.tt("dve", vr[:], v4(ps[:, 0:256]), bc(self.kapR, [128, 4, 64], 2), ALU.mult, [ps, cc], [vr])
                        yield
                        sigm(sgr[:], ps[:, 256:512], ps, sgr)
                        yield
                        self.tt("dve", sgr[:], sgr[:], ps[:, 256:512], ALU.mult, [sgr, ps], [sgr])
                        self.prel(ps)
                        self.tt("pool", gsg[:], sgr[:], rr[:, RR_RETG:RR_RETG + 256], ALU.mult, [sgr, rr], [gsg])
                        yield
                        yield from self.wf('b')
                        ptb = self.psb()
                        for p in range(2):
                            self.tr(ptb[:, p * 128:(p + 1) * 128], rkT[:, p, csl], [rkT], [ptb])
                        self.cp("act", ktr[:], ptb[:, 0:256], [ptb], [ktr])
                        self.prel(ptb)
                        yield
                        yield from self.lin_core(rqT, rkT, csl, ktr, vr, 64, Sr, Sr16, self.EMr.rearrange("p (a b) -> p a b", a=2), [cc], pTs_["R"])
                        pso = self.last_pso["R"] if False else self._pso
                        self.tt("dve", v4(o1[:]), v4(pso[:, 0:256]), bc(self.qdec, [128, 4, 64], 2), ALU.mult, [pso, cc], [o1])
                        self.prel(pso)
                        yield
                        yield from self.head_norm(o1, o2, o3, sm, gsg, ytok["R"])

                    def sec_ssd():
                        sm = smS
                        o1, o2, o3 = ob["S"]
                        self.tt("dve", sm[:, 8:12], st4[:, 0:4], rr[:, RR_DTB:RR_DTB + 4], ALU.add, [st4, rr], [sm])
                        self.act(sm[:, 12:16], sm[:, 8:12], AF.Exp, [sm], [sm])
                        yield
                        self.act(sm[:, 16:20], sm[:, 12:16], AF.Ln, [sm], [sm], bias=1.0)
                        yield
                        self.tt("dve", sm[:, 20:24], sm[:, 16:20], arep[:], ALU.mult, [sm, arep], [sm])
                        yield
                        yield from self.wf('f')
                        psA = self.psf()
                        self.mm(psA[:, 0:4], tri, sm[:, 20:24], True, True, [cc, sm], [psA])
                        self.mm(psA[:, 4:8], ones, sm[:, 20:24], True, True, [cc, sm], [psA])
                        self.tt("dve", Rb[:], bc(tri, [128, 4, 128], 1), bc(sm[:, 20:24], [128, 4, 128], 2), ALU.mult, [cc, sm], [Rb])
                        yield
                        self.cp("act", sm[:, 24:32], psA[:, 0:8], [psA], [sm])
                        self.prel(psA)
                        yield from self.wf('f')
                        psR = self.psf()
                        self.mm(psR[:], ones, Rb[:].rearrange("p h i -> p (h i)"), True, True, [cc, Rb], [psR])
                        yield
                        self.tt("dve", Dm[:], psR[:].rearrange("p (h i) -> p h i", h=4), bc(sm[:, 24:28], [128, 4, 128], 2), ALU.subtract, [psR, sm], [Dm])
                        self.prel(psR)
                        yield
                        self.tt("pool", Dm[:], Dm[:], bc(neg, [128, 4, 128], 1), ALU.add, [Dm, cc], [Dm])
                        yield
                        self.act(Dm[:], Dm[:], AF.Exp, [Dm], [Dm])
                        yield from self.wf('b')
                        ptb = self.psb()
                        for j in range(4):
                            self.tr(ptb[:, j * 128:(j + 1) * 128], cvT[:, j, csl], [cvT], [ptb])
                        self.cp("act", xBt[:], ptb[:, 0:512].rearrange("p (a n) -> p a n", a=4), [ptb], [xBt])
                        self.prel(ptb)
                        yield
                        xtok = xBt[:, 0:2, :].rearrange("p a (r v) -> p (a r) v", r=2)
                        self.tt("dve", vs[:], xtok, bc(sm[:, 16:20], [128, 4, 64], 2), ALU.mult, [xBt, sm], [vs])
                        self.tt("dve", sm[:, 32:36], sm[:, 28:32], sm[:, 24:28], ALU.subtract, [sm], [sm])
                        yield
                        self.act(sm[:, 36:40], sm[:, 32:36], AF.Exp, [sm], [sm])
                        self.act(sm[:, 40:48], sm[:, 24:32], AF.Exp, [sm], [sm])
                        yield from self.wf('f')
                        pss = self.psf()
                        for g in range(2):
                            self.mm(pss[:, g * 128:(g + 1) * 128], cvT[:, 2 + g, csl], cvT[:, 4 + g, csl], True, True, [cvT], [pss])
                        yield
                        self.tt("dve", vs2[:], vs[:], bc(sm[:, 36:40], [128, 4, 64], 2), ALU.mult, [vs, sm], [vs2])
                        pTs = pTs_["S"]
                        for g in range(2):
                            self.tt("dve", pTs[:, 2 * g:2 * g + 2, :], bc(pss[:, g * 128:(g + 1) * 128], [128, 2, 128], 1), Dm[:, 2 * g:2 * g + 2, :], ALU.mult, [pss, Dm], [pTs])
                        self.prel(pss)
                        yield
                        yield from self.wf('f', 2)
                        psy = self.psf()
                        for h in range(4):
                            self.mm(psy[:, h * 64:(h + 1) * 64], pTs[:, h, :], vs[:, h, :], True, True, [pTs, vs], [psy])
                        for g in range(2):
                            self.mm(psy[:, 256 + g * 128:256 + (g + 1) * 128], cvT[:, 4 + g, csl], Ss16[:, 2 * g:2 * g + 2, :].rearrange("p a v -> p (a v)"), True, True, [cvT, Ss16], [psy])
                        psu = self.psf()
                        for g in range(2):
                            self.mm(psu[:, g * 128:(g + 1) * 128], xBt[:, 2 + g, :], vs2[:, 2 * g:2 * g + 2, :].rearrange("p a v -> p (a v)"), True, True, [xBt, vs2], [psu])
                        yield
                        self.tt("dve", Ss[:], Ss[:], bc(sm[:, 44:48], [128, 4, 64], 2), ALU.mult, [Ss, sm], [Ss])
                        yield
                        self.tt("dve", Ss[:], Ss[:], v4(psu[:, 0:256]), ALU.add, [Ss, psu], [Ss])
                        self.prel(psu)
                        yield
                        self.cp("act", Ss16[:], Ss[:], [Ss], [Ss16])
                        self.tt("dve", v4(o1[:]), v4(psy[:, 256:512]), bc(sm[:, 40:44], [128, 4, 64], 2), ALU.mult, [psy, sm], [o1])
                        yield
                        self.tt("dve", o1[:], o1[:], psy[:, 0:256], ALU.add, [o1, psy], [o1])
                        self.prel(psy)
                        self.tt("pool", v4(o2[:]), xtok, v4(rr[:, RR_DVEC:RR_DVEC + 256]), ALU.mult, [xBt, rr], [o2])
                        yield
                        self.tt("dve", o1[:], o1[:], o2[:], ALU.add, [o1, o2], [o1])
                        yield
                        self.tt("dve", o1[:], o1[:], sgz[:, 0:256], ALU.mult, [o1, sgz], [o1])
                        yield
                        self.act(o3[:], o1[:], AF.Square, [o1], [o3, sm], accum=sm[:, 48:49])
                        yield
                        rstd(sm[:, 49:50], sm[:, 48:49], 1.0 / 256, sm)
                        yield
                        self.stt("dve", ytok["S"][:], o1[:], sm[:, 49:50], rr[:, RR_SSDG:RR_SSDG + 256], ALU.mult, ALU.mult, [o1, sm, rr], [ytok["S"]])

                    def sec_gla():
                        sm = smG
                        o1, o2, o3 = ob["G"]
                        gsg = gsgs["G"]
                        yield from self.wf('f')
                        psa = self.psf()
                        self.mm(psa[:, 0:128], gaT[0:33, csl], wal[:], True, True, [gaT, wal], [psa])
                        self.act(e1[:], psa[:, 0:128], AF.Exp, [psa], [e1], scale=-1.0)
                        self.prel(psa)
                        yield
                        self.act(e1[:], e1[:], AF.Ln, [e1], [e1], bias=1.0)
                        yield
                        self.ts("dve", lap[:, :, 0:32], e1[:].rearrange("p (h d) -> p h d", h=4), -1.0 / 16.0, None, ALU.mult, None, [e1], [lap])
                        self.tt("pool", gsg[:], sgz[:, 256:512], rr[:, RR_GLAG:RR_GLAG + 256], ALU.mult, [sgz, rr], [gsg])
                        yield
                        yield from self.wf('f')
                        psg = self.psf()
                        self.mm(psg[:, 0:256], tri, lap[:].rearrange("p h d -> p (h d)"), True, True, [cc, lap], [psg])
                        for p in range(2):
                            self.mm(psg[:, 256 + p:257 + p], lap[:, 2 * p:2 * p + 2, :].rearrange("p a d -> p (a d)"), ones[:, 0:1], True, True, [lap, cc], [psg])
                        yield
                        self.act(eq[:], v4(psg[:, 0:256]), AF.Exp, [psg], [eq])
                        self.act(ek[:], v4(psg[:, 0:256]), AF.Exp, [psg], [ek], scale=-1.0)
                        self.act(sm[:, 0:2], psg[:, 256:258], AF.Exp, [psg], [sm])
                        self.prel(psg)
                        yield
                        for p in range(2):
                            self.ts("dve", EMg[:, p, :], bd01, sm[:, p:p + 1], None, ALU.mult, None, [cc, sm], [EMg])
                        yield from self.wf('f')
                        ps1 = tmproj(1, 512)
                        yield
                        self.stt("dve", qpad[:, :, 0:32], ps1[:, 0:128].rearrange("p (h d) -> p h d", h=4), 32.0 ** -0.5, eq[:, :, 0:32], ALU.mult, ALU.mult, [ps1, eq], [qpad])
                        yield
                        self.tt("dve", kpad[:, :, 0:32], ps1[:, 128:256].rearrange("p (h d) -> p h d", h=4), ek[:, :, 0:32], ALU.mult, [ps1, ek], [kpad])
                        yield
                        self.cp("act", vg[:], v4(ps1[:, 256:512]), [ps1], [vg])
                        self.prel(ps1)
                        yield from self.wf('b')
                        ptb = self.psb()
                        for p in range(2):
                            self.tr(ptb[:, p * 128:(p + 1) * 128], qpad[:, 2 * p:2 * p + 2, :].rearrange("p a d -> p (a d)"), [qpad], [ptb])
                            self.tr(ptb[:, 256 + p * 128:256 + (p + 1) * 128], kpad[:, 2 * p:2 * p + 2, :].rearrange("p a d -> p (a d)"), [kpad], [ptb])
                        self.cp("act", qkTg[:], ptb[:, 0:512].rearrange("p (a n) -> p a n", a=4), [ptb], [qkTg])
                        self.prel(ptb)
                        yield
                        yield from self.lin_core(qkTg, qkTg, None, kpad[:].rearrange("p h d -> p (h d)"), vg, 64, Sg, Sg16, EMg[:], [EMg], pTs_["G"], koff=2, ktok_buf=kpad)
                        pso = self._pso
                        self.cp("act", o1[:], pso[:, 0:256], [pso], [o1])
                        self.prel(pso)
                        yield
                        yield from self.head_norm(o1, o2, o3, sm, gsg, ytok["G"])

                    def sec_ml():
                        sm = smM
                        o1, o2, o3 = ob["M"]
                        gsg = gsgs["M"]
                        yield from self.wf('f')
                        ps2 = tmproj(2, 512)
                        sigm(sgm[:], ps2[:, 256:512], ps2, sgm)
                        yield
                        self.tt("pool", gsg[:], sgm[:], rr[:, RR_MLG:RR_MLG + 256], ALU.mult, [sgm, rr], [gsg])
                        self.tt("dve", sm[:, 8:12], st4[:, 8:12], rr[:, RR_BF:RR_BF + 4], ALU.add, [st4, rr], [sm])
                        yield
                        self.act(sm[:, 12:16], sm[:, 8:12], AF.Exp, [sm], [sm], scale=-1.0)
                        yield
                        self.act(sm[:, 16:20], sm[:, 12:16], AF.Ln, [sm], [sm], bias=1.0)
                        yield
                        yield from self.wf('f')
                        psb_ = self.psf()
                        self.mm(psb_[:, 0:4], tri, sm[:, 16:20], True, True, [cc, sm], [psb_])
                        self.mm(psb_[:, 4:8], ones, sm[:, 16:20], True, True, [cc, sm], [psb_])
                        self.tt("dve", sm[:, 20:24], st4[:, 4:8], rr[:, RR_BI:RR_BI + 4], ALU.add, [st4, rr], [sm])
                        yield
                        self.tt("dve", sm[:, 20:24], sm[:, 20:24], psb_[:, 0:4], ALU.add, [sm, psb_], [sm])
                        yield
                        self.act(sm[:, 24:28], sm[:, 20:24], AF.Exp, [sm], [sm], bias=math.log(0.125))
                        self.act(sm[:, 28:36], psb_[:, 0:8], AF.Exp, [psb_], [sm], scale=-1.0)
                        self.prel(psb_)
                        yield
                        self.tt("dve", vm[:, :, 0:64], v4(ps2[:, 0:256]), bc(sm[:, 24:28], [128, 4, 64], 2), ALU.mult, [ps2, sm], [vm])
                        self.prel(ps2)
                        yield
                        self.cp("dve", vm[:, :, 64:65], sm[:, 24:28].unsqueeze(2), [sm], [vm])
                        for p in range(2):
                            for r in range(2):
                                hh = 2 * p + r
                                self.ts("dve", EMm[r * 64:(r + 1) * 64, p, r * 65:(r + 1) * 65], ones[r * 64:(r + 1) * 64, 0:65], sm[r * 64:(r + 1) * 64, 32 + hh:33 + hh], None, ALU.mult, None, [cc, sm], [EMm])
                            yield
                        yield from self.wf('b')
                        ptb = self.psb()
                        for p in range(2):
                            self.tr(ptb[:, p * 128:(p + 1) * 128], cvT[:, 8 + p, csl], [cvT], [ptb])
                        self.cp("act", ktm[:], ptb[:, 0:256], [ptb], [ktm])
                        self.prel(ptb)
                        yield
                        yield from self.lin_core(cvT, cvT, csl, ktm, vm, 65, Sm, Sm16, EMm[:], [EMm], pTs_["M"], qoff=6, koff=8)
                        pso = self._pso
                        pso3 = pso[:, 0:260].rearrange("p (h v) -> p h v", h=4)
                        self.tt("dve", sm[:, 36:40], pso3[:, :, 64], sm[:, 28:32], ALU.mult, [pso, sm], [sm])
                        yield
                        self.stt("dve", sm[:, 40:44], sm[:, 36:40], -1.0, sm[:, 36:40], ALU.mult, ALU.max, [sm], [sm])
                        yield
                        self.ts("dve", sm[:, 40:44], sm[:, 40:44], 1.0, None, ALU.max, None, [sm], [sm])
                        yield
                        self.P.op("dve", lambda e: e.reciprocal(out=sm[:, 44:48], in_=sm[:, 40:44]), [sm], [sm])
                        yield
                        self.tt("dve", sm[:, 44:48], sm[:, 44:48], sm[:, 28:32], ALU.mult, [sm], [sm])
                        yield
                        self.tt("dve", v4(o1[:]), pso3[:, :, 0:64], bc(sm[:, 44:48], [128, 4, 64], 2), ALU.mult, [pso, sm], [o1])
                        self.prel(pso)
                        yield
                        yield from self.head_norm(o1, o2, o3, sm, gsg, ytok["M"])

                    gens = []
                    if 'ret' in self.en:
                        gens.append(sec_ret())
                    if 'ssd' in self.en:
                        gens.append(sec_ssd())
                    if 'gla' in self.en:
                        gens.append(sec_gla())
                    if 'ml' in self.en:
                        gens.append(sec_ml())
                    while gens:
                        for g_ in list(gens):
                            try:
                                next(g_)
                            except StopIteration:
                                gens.remove(g_)

                    ptb = self.psb()
                    for j, m in enumerate("RSGM"):
                        for vc in range(2):
                            self.tr(ptb[:, (2 * j + vc) * 128:(2 * j + vc + 1) * 128], ytok[m][:, vc * 128:(vc + 1) * 128], [ytok[m]], [ptb])
                    self.cp("act", yT[:, :, csl], ptb[:].rearrange("p (k n) -> p k n", k=8), [ptb], [yT])
                    self.prel(ptb)
                P.dma("sp", self.yT_d[:, :, tsl].rearrange("k p n -> p k n"), yT[:], reads=[yT])

    def lin_core(self, qT, kT, csl, ktok, v, dv, S_, S16, EM, EMb, pTb, qoff=0, koff=0, ktok_buf=None):
        tri = self.tri
        cc = self.cc
        if csl is None:
            qa = lambda p, r: qT[r * 64:(r + 1) * 64, qoff + p, :]
            ka = lambda p, r: kT[r * 64:(r + 1) * 64, koff + p, :]
        else:
            qa = lambda p, r: qT[r * 64:(r + 1) * 64, qoff + p, csl]
            ka = lambda p, r: kT[r * 64:(r + 1) * 64, koff + p, csl]
        yield from self.wf('f', 2)
        pssr = [self.psf(), self.psf()]
        for h in range(4):
            p, r = h // 2, h % 2
            self.mm(pssr[r][:, p * 128:(p + 1) * 128], ka(p, r), qa(p, r), True, True, [kT, qT], [pssr[r]])
        yield
        for r in range(2):
            self.tt("dve", pTb[:, r:4:2, :], pssr[r][:, 0:256].rearrange("p (a i) -> p a i", a=2), bc(tri, [128, 2, 128], 1), ALU.mult, [pssr[r], cc], [pTb])
            self.prel(pssr[r])
            yield
        yield from self.wf('f', 2)
        pso = self.psf()
        for h in range(4):
            p, r = h // 2, h % 2
            self.mm(pso[:, h * dv:(h + 1) * dv], pTb[:, h, :], v[:, h, :], True, False, [pTb, v], [pso])
            self.mm(pso[:, h * dv:(h + 1) * dv], qa(p, r), S16[r * 64:(r + 1) * 64, p, r * dv:(r + 1) * dv], False, True, [qT, S16], [pso])
        psu = self.psf()
        kb = ktok_buf if ktok_buf is not None else ktok
        for p in range(2):
            self.mm(psu[:, p * 2 * dv:(p + 1) * 2 * dv], ktok[:, p * 128:(p + 1) * 128], v[:, 2 * p:2 * p + 2, :].rearrange("p a v -> p (a v)"), True, True, [kb, v], [psu])
        yield
        self.tt("dve", S_[:], S_[:], psu[:, 0:4 * dv].rearrange("p (a v) -> p a v", a=2), ALU.add, [S_, psu], [S_])
        self.prel(psu)
        yield
        self.tt("dve", S_[:], S_[:], EM, ALU.mult, [S_] + list(EMb), [S_])
        yield
        self.cp("act", S16[:], S_[:], [S_], [S16])
        self._pso = pso

    def head_norm(self, o1, o2, o3, sm, gsg, yout):
        self.act(o2[:], o1[:], AF.Square, [o1], [o2])
        yield
        self.P.op("dve", lambda e: e.tensor_reduce(out=sm[:, 4:8], in_=o2[:].rearrange("p (h v) -> p h v", h=4), axis=AX.X, op=ALU.add), [o2], [sm])
        yield
        self.act(sm[:, 4:8], sm[:, 4:8], AF.Ln, [sm], [sm], scale=1.0 / 64, bias=EPS)
        yield
        self.act(sm[:, 4:8], sm[:, 4:8], AF.Exp, [sm], [sm], scale=-0.5)
        yield
        self.tt("dve", o3[:].rearrange("p (h v) -> p h v", h=4), o1[:].rearrange("p (h v) -> p h v", h=4), bc(sm[:, 4:8], [128, 4, 64], 2), ALU.mult, [o1, sm], [o3])
        yield
        self.tt("dve", yout[:], o3[:], gsg[:], ALU.mult, [o3, gsg], [yout])


    def phaseB(self, l, xsrc):
        nc, P, S, NT = self.nc, self.P, self.S, self.NT
        moe = (l % 2 == 1)
        with ExitStack() as st:
            sb = lambda name, shape, dt=F32: self.sb(st, name, shape, dt)
            wG = [sb("wG%d" % k, [128, 4096], BF16) for k in range(8)]
            for k in range(8):
                P.dma("pool", wG[k][:], self.w_in[l, k * 128:(k + 1) * 128, 3868:7964], writes=[wG[k]])
            wb = [sb("wb%d" % j, [128, D], BF16) for j in range(8)]
            for j in range(8):
                n_, vc = j // 2, j % 2
                P.dma("pool", wb[j][:], self.wbr[l, n_, vc * 128:(vc + 1) * 128, :], writes=[wb[j]])
            wo = [sb("wo%d" % k, [128, D], BF16) for k in range(8)]
            for k in range(8):
                P.dma("pool", wo[k][:], self.wout[l, k * 128:(k + 1) * 128, :], writes=[wo[k]])
            gr = sb("grB", [128, D])
            P.dma("sp", gr[:], self.rowrep[l, :, RR_FFNG:RR_FFNG + D], writes=[gr])
            if moe:
                wr = sb("wr", [128, 8, NE], BF16)
                P.dma("pool", wr[:], self.rw[l // 2].rearrange("(k p) e -> p k e", p=128), writes=[wr])
            hT = sb("hTB", [128, 8, 512], BF16)
            yT = sb("yTB", [128, 8, 512], BF16)
            mg = sb("mg", [128, 8, 512], BF16)
            sg = [sb("sg%d" % i, [128, 512]) for i in range(2)]
            macc = sb("macc", [128, 512])
            xt = [sb("xtB%d" % i, [128, D]) for i in range(2)]
            sqj = sb("sqjB", [128, D], BF16)
            hb = sb("hbB", [128, D], BF16)
            hT2 = sb("hT2B", [128, 8, 512], BF16)
            sm = sb("smB", [128, 64])
            lg = sb("lg", [128, 4, NE])
            for t in range(NT):
                tsl = slice(t * 512, (t + 1) * 512)
                P.dma("sp", hT[:], self.hT_d[:, :, tsl].rearrange("k p n -> p k n"), writes=[hT])
                P.dma("sp", yT[:], self.yT_d[:, :, tsl].rearrange("k p n -> p k n"), writes=[yT])
                for dc in range(8):
                    for n_ in range(4):
                        psg = self.psf()
                        for k in range(8):
                            self.mm(psg[:], wG[k][:, n_ * D + dc * 128:n_ * D + (dc + 1) * 128], hT[:, k, :], k == 0, k == 7, [wG[k], hT], [psg])
                        psb_ = self.psf()
                        for vc in range(2):
                            self.mm(psb_[:], wb[n_ * 2 + vc][:, dc * 128:(dc + 1) * 128], yT[:, n_ * 2 + vc, :], vc == 0, vc == 1, [wb[n_ * 2 + vc], yT], [psb_])
                        s_ = sg[n_ % 2]
                        self.act(s_[:], psg[:], AF.Sigmoid, [psg], [s_])
                        self.prel(psg)
                        if n_ == 0:
                            self.tt("dve", macc[:], s_[:], psb_[:], ALU.mult, [s_, psb_], [macc])
                            self.prel(psb_)
                        else:
                            self.tt("dve", s_[:], s_[:], psb_[:], ALU.mult, [s_, psb_], [s_])
                            self.prel(psb_)
                            if n_ < 3:
                                self.tt("pool", macc[:], macc[:], s_[:], ALU.add, [macc, s_], [macc])
                            else:
                                self.tt("dve", mg[:, dc, :], macc[:], s_[:], ALU.add, [macc, s_], [mg])
                for c in range(4):
                    row0 = (t * 4 + c) * 128
                    csl = slice(c * 128, (c + 1) * 128)
                    x_ = xt[c % 2]
                    P.dma("sp", x_[:], xsrc[row0:row0 + 128, :], writes=[x_])
                    for hf in range(2):
                        ps = self.psf()
                        for k in range(8):
                            self.mm(ps[:], mg[:, k, csl], wo[k][:, hf * 512:(hf + 1) * 512], k == 0, k == 7, [mg, wo[k]], [ps])
                        self.tt("dve", x_[:, hf * 512:(hf + 1) * 512], x_[:, hf * 512:(hf + 1) * 512], ps[:], ALU.add, [x_, ps], [x_])
                        self.prel(ps)
                    P.dma("sp", self.xres[row0:row0 + 128, :], x_[:], reads=[x_])
                    self.act(sqj[:], x_[:], AF.Square, [x_], [sqj, sm], accum=sm[:, 0:1])
                    self.act(sm[:, 1:2], sm[:, 0:1], AF.Sqrt, [sm], [sm], scale=1.0 / D, bias=EPS)
                    self.P.op("dve", lambda e: e.reciprocal(out=sm[:, 2:3], in_=sm[:, 1:2]), [sm], [sm])
                    self.stt("dve", hb[:], x_[:], sm[:, 2:3], gr[:], ALU.mult, ALU.mult, [x_, sm, gr], [hb])
                    pt = self.psb()
                    for k in range(8):
                        self.tr(pt[:, k * 128:(k + 1) * 128], hb[:, k * 128:(k + 1) * 128], [hb], [pt])
                    self.cp("act", hT2[:, :, csl], pt[:].rearrange("p (k n) -> p k n", k=8), [pt], [hT2])
                    self.prel(pt)
                    if moe:
                        ps = self.psf()
                        for k in range(8):
                            self.mm(ps[:, 0:NE], hT2[:, k, csl], wr[:, k, :], k == 0, k == 7, [hT2, wr], [ps])
                        self.cp("act", lg[:, c, :], ps[:, 0:NE], [ps], [lg])
                        self.prel(ps)
                P.dma("sp", self.hT2_d[:, :, tsl].rearrange("k p n -> p k n"), hT2[:], reads=[hT2])
                if moe:
                    self.top2(lg, sm, self.gate[:, t * 4:(t + 1) * 4, :], st, t)

    def top2(self, lg, sm, gout, st, t):
        if t == 0:
            self.t2 = [self.sb(st, "t2_%d" % i, [128, 4, NE], F32) for i in range(4)]
            self.t2s = self.sb(st, "t2s", [128, 16], F32)
        m1b, l2, m2b, tmp = self.t2
        s_ = self.t2s
        gate = self.gate
        red = lambda out, in_: self.P.op("dve", lambda e: e.tensor_reduce(out=out, in_=in_, axis=AX.X, op=ALU.max), [lg, l2], [s_])
        red(s_[:, 0:4], lg[:])
        self.tt("dve", m1b[:], lg[:], bc(s_[:, 0:4], [128, 4, NE], 2), ALU.is_equal, [lg, s_], [m1b])
        self.stt("dve", l2[:], m1b[:], -1e30, lg[:], ALU.mult, ALU.add, [m1b, lg], [l2])
        red(s_[:, 4:8], l2[:])
        self.tt("dve", m2b[:], l2[:], bc(s_[:, 4:8], [128, 4, NE], 2), ALU.is_equal, [l2, s_], [m2b])
        self.tt("dve", s_[:, 8:12], s_[:, 4:8], s_[:, 0:4], ALU.subtract, [s_], [s_])
        self.act(s_[:, 8:12], s_[:, 8:12], AF.Exp, [s_], [s_])
        self.ts("dve", s_[:, 8:12], s_[:, 8:12], 1.0, None, ALU.add, None, [s_], [s_])
        self.P.op("dve", lambda e: e.reciprocal(out=s_[:, 12:16], in_=s_[:, 8:12]), [s_], [s_])
        self.ts("dve", s_[:, 8:12], s_[:, 12:16], -1.0, 1.0, ALU.mult, ALU.add, [s_], [s_])
        self.tt("dve", m1b[:], m1b[:], bc(s_[:, 12:16], [128, 4, NE], 2), ALU.mult, [m1b, s_], [m1b])
        self.tt("dve", m2b[:], m2b[:], bc(s_[:, 8:12], [128, 4, NE], 2), ALU.mult, [m2b, s_], [m2b])
        self.tt("dve", gout, m1b[:], m2b[:], ALU.add, [m1b, m2b], [gate])

    def phaseC(self, l):
        nc, P, S, NT = self.nc, self.P, self.S, self.NT
        moe = (l % 2 == 1)
        li = l // 2
        if moe:
            items = [(e, hf) for e in range(NE) for hf in range(2)]
        else:
            items = [(None, 0), (None, 1)]
        HF = DFF // 2
        with ExitStack() as st:
            sb = lambda name, shape, dt=F32: self.sb(st, name, shape, dt)
            W1 = [[sb("W1_%d_%d" % (i, k), [128, HF], BF16) for k in range(8)] for i in range(2)]
            W3 = [[sb("W3_%d_%d" % (i, k), [128, HF], BF16) for k in range(8)] for i in range(2)]
            W2 = [[sb("W2_%d_%d" % (i, f), [128, D], BF16) for f in range(11)] for i in range(2)]
            hT2 = [sb("hT2C%d" % i, [128, 8, 512], BF16) for i in range(2)]
            h1T = sb("h1T", [128, 11, 512], BF16)
            gs = [sb("gs%d" % i, [128, 512]) for i in range(2)]
            xt = [sb("xtC%d" % i, [128, D]) for i in range(3)]
            xi = 0
            hi = 0

            def load_w(idx, slot):
                e, hf = items[idx]
                if e is None:
                    s1, s3, s2 = self.fw1[li], self.fw3[li], self.fw2[li]
                else:
                    s1, s3, s2 = self.mw1[li, e], self.mw3[li, e], self.mw2[li, e]
                for k in range(8):
                    P.dma("pool", W1[slot][k][:], s1[k * 128:(k + 1) * 128, hf * HF:(hf + 1) * HF], writes=[W1[slot][k]])
                    P.dma("pool", W3[slot][k][:], s3[k * 128:(k + 1) * 128, hf * HF:(hf + 1) * HF], writes=[W3[slot][k]])
                for f in range(11):
                    P.dma("pool", W2[slot][f][:], s2[hf * HF + f * 128:hf * HF + (f + 1) * 128, :], writes=[W2[slot][f]])

            load_w(0, 0)
            for idx, (e, hf) in enumerate(items):
                slot = idx % 2
                if idx + 1 < len(items):
                    load_w(idx + 1, (idx + 1) % 2)
                for t in range(NT):
                    tsl = slice(t * 512, (t + 1) * 512)
                    h_ = hT2[hi % 2]; hi += 1
                    P.dma("sp", h_[:], self.hT2_d[:, :, tsl].rearrange("k p n -> p k n"), writes=[h_])
                    for f in range(11):
                        pa = self.psf()
                        for k in range(8):
                            self.mm(pa[:], W1[slot][k][:, f * 128:(f + 1) * 128], h_[:, k, :], k == 0, k == 7, [W1[slot][k], h_], [pa])
                        pb_ = self.psf()
                        for k in range(8):
                            self.mm(pb_[:], W3[slot][k][:, f * 128:(f + 1) * 128], h_[:, k, :], k == 0, k == 7, [W3[slot][k], h_], [pb_])
                        g_ = gs[f % 2]
                        self.act(g_[:], pa[:], AF.Silu, [pa], [g_])
                        self.tt("dve", h1T[:, f, :], g_[:], pb_[:], ALU.mult, [g_, pb_], [h1T])
                        self.prel(pa, pb_)
                    for c in range(4):
                        row0 = (t * 4 + c) * 128
                        x_ = xt[xi % 3]; xi += 1
                        P.dma("sp", x_[:], self.xres[row0:row0 + 128, :], writes=[x_])
                        for hh in range(2):
                            ps = self.psf()
                            for f in range(11):
                                self.mm(ps[:], h1T[:, f, c * 128:(c + 1) * 128], W2[slot][f][:, hh * 512:(hh + 1) * 512], f == 0, f == 10, [h1T, W2[slot][f]], [ps])
                            if e is None:
                                self.tt("dve", x_[:, hh * 512:(hh + 1) * 512], x_[:, hh * 512:(hh + 1) * 512], ps[:], ALU.add, [x_, ps], [x_])
                            else:
                                self.stt("dve", x_[:, hh * 512:(hh + 1) * 512], ps[:], self.gate[:, t * 4 + c, e:e + 1], x_[:, hh * 512:(hh + 1) * 512], ALU.mult, ALU.add, [ps, self.gate, x_], [x_])
                            self.prel(ps)
                        P.dma("sp", self.xres[row0:row0 + 128, :], x_[:], reads=[x_])
                P.barrier(("sp",))

    def phaseD(self):
        P, S = self.P, self.S
        with ExitStack() as st:
            sb = lambda name, shape, dt=F32: self.sb(st, name, shape, dt)
            g = sb("gD", [128, D])
            P.dma("sp", g[:], self.fing, writes=[g])
            xt = [sb("xtD%d" % i, [128, D]) for i in range(2)]
            yo = [sb("yoD%d" % i, [128, D]) for i in range(2)]
            sqj = sb("sqjD", [128, D], BF16)
            sm = sb("smD", [128, 8])
            for c in range(S // 128):
                x_ = xt[c % 2]
                y_ = yo[c % 2]
                P.dma("sp", x_[:], self.xres[c * 128:(c + 1) * 128, :], writes=[x_])
                self.act(sqj[:], x_[:], AF.Square, [x_], [sqj, sm], accum=sm[:, 0:1])
                self.act(sm[:, 1:2], sm[:, 0:1], AF.Sqrt, [sm], [sm], scale=1.0 / D, bias=EPS)
                self.P.op("dve", lambda e: e.reciprocal(out=sm[:, 2:3], in_=sm[:, 1:2]), [sm], [sm])
                self.stt("dve", y_[:], x_[:], sm[:, 2:3], g[:], ALU.mult, ALU.mult, [x_, sm, g], [y_])
                P.dma("sp", self.out[c * 128:(c + 1) * 128, :], y_[:], reads=[y_])


def _consts(S):
    j = np.arange(128)
    tri = (j[:, None] <= j[None, :]).astype(np.float32)
    neg = np.where(j[:, None] <= j[None, :], 0.0, -1e30).astype(np.float32)
    ones = np.ones((128, 128), np.float32)
    bd = ((j[:, None] // 64) == (j[None, :] // 64)).astype(np.float32)
    lg = np.log1p(-np.exp2(-5.0 - np.arange(4, dtype=np.float64)))
    kap = (np.exp(-(j[:, None] + 1.0) * lg[None, :]) * 0.125).astype(np.float32)
    qd = np.exp((j[:, None] + 1.0) * lg[None, :]).astype(np.float32)
    em = np.zeros((128, 2, 128), np.float32)
    for p in range(2):
        for r in range(2):
            em[r * 64:(r + 1) * 64, p, r * 64:(r + 1) * 64] = np.exp(128.0 * lg[2 * p + r])
    c32 = np.concatenate([tri, neg, ones, bd, np.zeros((128, 128), np.float32), kap, qd, em.reshape(128, 256)], axis=1)
    half = 32
    inv = (10000.0 ** (-np.arange(half, dtype=np.float32) / half)).astype(np.float32)
    ang = np.arange(S, dtype=np.float32)[None, :] * inv[:, None]
    cos = np.cos(ang).astype(np.float32)
    sin = np.sin(ang).astype(np.float32)
    cosT = np.concatenate([cos, cos, cos, cos], axis=0)
    sinT = np.concatenate([-sin, sin, -sin, sin], axis=0)
    ident = np.eye(128, dtype=np.float32).astype(ml_dtypes.bfloat16)
    return dict(c32=np.ascontiguousarray(c32), cosT=np.ascontiguousarray(cosT), sinT=np.ascontiguousarray(sinT), identd=ident)


def _prep_weights(inp, L):
    w_in = inp["w_in"][:L]
    o = np.cumsum([0, 256, 256, 256, 256, 256, 768, 4, 128, 128, 256, 256, 16, 512, 256, 256, 4, 4, 4096])
    (RQ, RK, RV, RG, SZ, SX, SDT, GQ, GK, GV, GR, GA, MQK, MV, MO, MI, MF, MG) = o[:18]

    def swap(c0):
        idx = []
        for h in range(4):
            idx += list(range(c0 + h * 64 + 32, c0 + h * 64 + 64)) + list(range(c0 + h * 64, c0 + h * 64 + 32))
        return idx
    cols = (list(range(RQ, RQ + 256)) + swap(RQ) + list(range(RK, RK + 256)) + swap(RK)
            + list(range(SX, SX + 768)) + list(range(MQK, MQK + 512)) + list(range(GA, GA + 16))
            + list(range(RV, RV + 256)) + list(range(RG, RG + 256))
            + list(range(GQ, GQ + 128)) + list(range(GK, GK + 128)) + list(range(GV, GV + 256))
            + list(range(MV, MV + 256)) + list(range(MO, MO + 256))
            + list(range(SZ, SZ + 256)) + list(range(GR, GR + 256))
            + list(range(SDT, SDT + 4)) + list(range(MI, MI + 4)) + list(range(MF, MF + 4)))
    assert len(cols) == NA
    wA = np.ascontiguousarray(w_in[:, :, cols])
    rowrep = np.zeros((L, NRR), np.float32)
    rowrep[:, RR_MIXG:RR_MIXG + D] = inp["mix_norm_g"][:L]
    rowrep[:, RR_FFNG:RR_FFNG + D] = inp["ffn_norm_g"][:L]
    rowrep[:, RR_RETG:RR_RETG + 256] = inp["ret_norm_g"][:L]
    rowrep[:, RR_SSDG:RR_SSDG + 256] = inp["ssd_norm_g"][:L]
    rowrep[:, RR_GLAG:RR_GLAG + 256] = inp["gla_norm_g"][:L]
    rowrep[:, RR_MLG:RR_MLG + 256] = inp["ml_norm_g"][:L]
    rowrep[:, RR_DVEC:RR_DVEC + 256] = np.repeat(inp["ssd_d"][:L], 64, axis=1)
    rowrep[:, RR_DTB:RR_DTB + 4] = inp["ssd_dt_bias"][:L]
    rowrep[:, RR_ALOG:RR_ALOG + 4] = inp["ssd_a_log"][:L]
    rowrep[:, RR_BI:RR_BI + 4] = inp["ml_b_i"][:L]
    rowrep[:, RR_BF:RR_BF + 4] = inp["ml_b_f"][:L]
    rowrep = np.ascontiguousarray(np.broadcast_to(rowrep[:, None, :], (L, 128, NRR)))
    colpack = np.zeros((L, 128, NCP), np.float32)
    cw = np.concatenate([inp["ssd_conv_w"][:L], inp["ml_conv_w"][:L]], axis=2)
    cb = np.concatenate([inp["ssd_conv_b"][:L], inp["ml_conv_b"][:L]], axis=1)
    colpack[:, :, CP_CW:CP_CW + 40] = cw.reshape(L, 4, 10, 128).transpose(0, 3, 2, 1).reshape(L, 128, 40)
    colpack[:, :, CP_CB:CP_CB + 10] = cb.reshape(L, 10, 128).transpose(0, 2, 1)
    walaug = np.zeros((L, 33, 128), np.float32)
    walaug[:, 0:16, :] = inp["gla_w_alpha"][:L]
    walaug[:, 32, :] = inp["gla_b_alpha"][:L]
    fing = np.ascontiguousarray(np.broadcast_to(inp["final_norm_g"][None, :], (128, D)))
    return dict(wA=wA, rowrep=rowrep, colpack=colpack, walaug=walaug, fing=fing)


_CACHE = {}


def run(inputs, S, L, ncores):
    key = (S, L)
    if key not in _CACHE:
        nc = bass.Bass("TRN2", target_bir_lowering=False)
        Bld(nc, S, L).build()
        _CACHE[key] = nc
    nc = _CACHE[key]
    inp = {k: np.asarray(v) for k, v in inputs.items()}
    shared = dict(_consts(S))
    shared.update(_prep_weights(inp, L))
    ND = (L + 1) // 2
    NM = max(L // 2, 1)
    shared["w_in"] = inp["w_in"][:L]
    shared["w_branch"] = inp["w_branch"][:L]
    shared["w_out"] = inp["w_out"][:L]
    shared["ffn_w1"] = inp["ffn_w1"][:ND]
    shared["ffn_w3"] = inp["ffn_w3"][:ND]
    shared["ffn_w2"] = inp["ffn_w2"][:ND]
    shared["router_w"] = inp["router_w"][:NM]
    shared["moe_w1"] = inp["moe_w1"][:NM]
    shared["moe_w3"] = inp["moe_w3"][:NM]
    shared["moe_w2"] = inp["moe_w2"][:NM]
    shared = {k: np.ascontiguousarray(v) for k, v in shared.items()}
    maps = []
    for c in range(ncores):
        m = dict(shared)
        m["x"] = np.ascontiguousarray(inp["x"][c, :S])
        maps.append(m)
    res = run_bass_kernel_spmd(nc, maps, core_ids=list(range(ncores)))
    return np.stack([res.results[c]["out"] for c in range(ncores)], axis=0)


def kernel(**inputs):
    return run(inputs, 4096, 4, 8).astype(np.float32)
```

```python
import math
from contextlib import ExitStack

import numpy as np
import ml_dtypes

import concourse.bass as bass
import concourse.mybir as mybir
from concourse.bass_utils import run_bass_kernel_spmd

F32 = mybir.dt.float32
BF16 = mybir.dt.bfloat16
AF = mybir.ActivationFunctionType
ALU = mybir.AluOpType
AX = mybir.AxisListType

D = 1024
NCOL = 7964
DFF = 2816
NE = 8
EPS = 1e-6
NDS = 64

FMW = 18 * 128 + 16
TMO = FMW
NA = FMW + 2048 + 12
RR_MIXG = 0
RR_FFNG = 1024
RR_RETG = 2048
RR_SSDG = 2304
RR_GLAG = 2560
RR_MLG = 2816
RR_DVEC = 3072
RR_DTB = 3328
RR_ALOG = 3332
RR_BI = 3336
RR_BF = 3340
NRR = 3344
CP_CW = 0
CP_CB = 40
NCP = 50


class Tk:
    __slots__ = ("w", "r")

    def __init__(self):
        self.w = None
        self.r = {}


class Buf:
    def __init__(self, t, excl=False):
        self.t = t
        self.k = Tk()
        self.excl = excl

    def __getitem__(self, idx):
        return self.t[idx]


class Prog:
    ENG = ("pe", "act", "dve", "pool", "sp")

    def __init__(self, nc, stack):
        self.nc = nc
        self.ops = {e: [] for e in self.ENG}
        self.sems = []
        self.esem = {}
        for e in ("pe", "act", "dve", "pool"):
            self.esem[e] = len(self.sems)
            self.sems.append(stack.enter_context(nc.semaphore("s_" + e)))
        self.cur = [0] * 4
        self.dsem = []
        for i in range(NDS):
            self.dsem.append(len(self.sems))
            self.sems.append(stack.enter_context(nc.semaphore("d%d" % i)))
            self.cur.append(0)
        self.dnext = {"sp": 0, "pool": 0}
        self.drange = {"sp": (0, 24), "pool": (24, NDS)}
        self.seen = {e: {} for e in self.ENG}

    def _waits(self, eng, reads, writes, extra=()):
        need = {}
        for t in reads:
            if t.w is not None:
                k, v = t.w
                if need.get(k, 0) < v:
                    need[k] = v
        for t in writes:
            if t.w is not None:
                k, v = t.w
                if need.get(k, 0) < v:
                    need[k] = v
            for k, v in t.r.items():
                if need.get(k, 0) < v:
                    need[k] = v
        for k, v in extra:
            if need.get(k, 0) < v:
                need[k] = v
        seen = self.seen[eng]
        out = []
        pe_own = self.esem["pe"]
        for k, v in need.items():
            if eng == "pe" and k == pe_own:
                continue
            if seen.get(k, 0) >= v:
                continue
            seen[k] = v
            out.append((k, v))
        return out

    def _track(self, ev, reads, writes):
        k, v = ev
        for t in reads:
            if t.r.get(k, 0) < v:
                t.r[k] = v
        for t in writes:
            t.w = ev
            t.r = {}

    region_on = False
    region_count = 0
    region_limit = 10 ** 9

    def op(self, eng, fn, reads=(), writes=()):
        if self.region_on:
            self.region_count += 1
            if self.region_count > self.region_limit:
                return
        writes = list(writes) + [b for b in reads if b.excl and b not in writes]
        reads = [b.k for b in reads]
        writes = [b.k for b in writes]
        waits = self._waits(eng, reads, writes)
        k = self.esem[eng]
        self.cur[k] += 1
        ev = (k, self.cur[k])
        self.ops[eng].append((waits, fn, k, 1))
        self._track(ev, reads, writes)

    def dma(self, q, out, in_, reads=(), writes=()):
        reads = [b.k for b in reads]
        writes = [b.k for b in writes]
        lo, hi = self.drange[q]
        j = lo + self.dnext[q]
        self.dnext[q] = (self.dnext[q] + 1) % (hi - lo)
        k = self.dsem[j]
        extra = [(k, self.cur[k])] if self.cur[k] > 0 else []
        waits = self._waits(q, reads, writes, extra)
        self.cur[k] += 16
        ev = (k, self.cur[k])
        self.ops[q].append((waits, (lambda e: e.dma_start(out=out, in_=in_)), k, 16))
        self._track(ev, reads, writes)

    def barrier(self, engs=None):
        for eng in (engs or self.ENG):
            seen = self.seen[eng]
            waits = []
            for k, v in enumerate(self.cur):
                if v > 0 and seen.get(k, 0) < v and not (eng == "pe" and k == self.esem["pe"]):
                    seen[k] = v
                    waits.append((k, v))
            self.ops[eng].append((waits, None, None, 0))

    def emit(self):
        nc = self.nc
        sems = self.sems

        def run(name, e):
            for waits, fn, sk, inc in self.ops[name]:
                for k, v in waits:
                    e.wait_ge(sems[k], v)
                if fn is None:
                    continue
                fn(e).then_inc(sems[sk], inc)

        with nc.Block() as block:
            @block.tensor
            def _(e):
                run("pe", e)

            @block.scalar
            def _(e):
                run("act", e)

            @block.vector
            def _(e):
                run("dve", e)

            @block.gpsimd
            def _(e):
                run("pool", e)

            @block.sync
            def _(e):
                run("sp", e)


def bc(ap, shape, axis):
    return ap.unsqueeze(axis).broadcast_to(list(shape))


class Bld:
    def __init__(self, nc, S, depth):
        self.nc = nc
        self.S = S
        self.depth = depth
        self.NT = S // 512
        import os
        self.en = set(os.environ.get('KSEC', 'ret,ssd,gla,ml').split(','))
        self.ph = os.environ.get('KPH', 'ABCD')

    def mm(self, out, lhsT, rhs, start, stop, rd, wr):
        self.P.op("pe", lambda e: e.matmul(out, lhsT=lhsT, rhs=rhs, start=start, stop=stop), rd, wr)

    def tr(self, out, in_, rd, wr):
        ident = self.ident
        self.P.op("pe", lambda e: e.transpose(out=out, in_=in_, identity=ident[:]), list(rd) + [ident], wr)

    def act(self, out, in_, func, rd, wr, scale=1.0, bias=None, accum=None):
        kw = {}
        if bias is not None:
            kw["bias"] = bias
        if accum is not None:
            kw["accum_out"] = accum
        self.P.op("act", lambda e: e.activation(out=out, in_=in_, func=func, scale=scale, **kw), rd, wr)

    def tt(self, eng, out, a, b, op, rd, wr):
        self.P.op(eng, lambda e: e.tensor_tensor(out=out, in0=a, in1=b, op=op), rd, wr)

    def ts(self, eng, out, a, s1, s2, op0, op1, rd, wr):
        if s2 is None:
            self.P.op(eng, lambda e: e.tensor_scalar(out=out, in0=a, scalar1=s1, scalar2=None, op0=op0), rd, wr)
        else:
            self.P.op(eng, lambda e: e.tensor_scalar(out=out, in0=a, scalar1=s1, scalar2=s2, op0=op0, op1=op1), rd, wr)

    def stt(self, eng, out, a, sc, b, op0, op1, rd, wr):
        self.P.op(eng, lambda e: e.scalar_tensor_tensor(out=out, in0=a, scalar=sc, in1=b, op0=op0, op1=op1), rd, wr)

    def cp(self, eng, out, in_, rd, wr):
        if eng == "act":
            self.P.op("act", lambda e: e.copy(out=out, in_=in_), rd, wr)
        else:
            self.P.op(eng, lambda e: e.tensor_copy(out=out, in_=in_), rd, wr)

    def ms(self, eng, ap, val, wr):
        self.P.op(eng, lambda e: e.memset(ap, val), [], wr)

    def psf(self):
        return self.pf_free.pop(0)

    def psb(self):
        return self.pb_free.pop(0)

    def prel(self, *bufs):
        for b in bufs:
            if b in self.pf:
                assert b not in self.pf_free
                self.pf_free.append(b)
            else:
                assert b not in self.pb_free
                self.pb_free.append(b)

    def wf(self, kind, n=1):
        pool = self.pf_free if kind == 'f' else self.pb_free
        while len(pool) < n:
            yield

    def sb(self, st, name, shape, dt):
        self.nid = getattr(self, "nid", 0) + 1
        return Buf(st.enter_context(self.nc.sbuf_tensor("%s_%d" % (name, self.nid), list(shape), dt)))

    def build(self):
        nc = self.nc
        S, L = self.S, self.depth
        ND = (L + 1) // 2
        NM = L // 2
        dr = lambda name, shape, dt=F32, kind="ExternalInput": nc.dram_tensor(name, list(shape), dt, kind=kind).ap()
        self.x_in = dr("x", [S, D])
        self.wA = dr("wA", [L, D, NA])
        self.w_in = dr("w_in", [L, D, NCOL])
        self.wbr = dr("w_branch", [L, 4, 256, D])
        self.wout = dr("w_out", [L, D, D])
        self.rowrep = dr("rowrep", [L, 128, NRR])
        self.colpack = dr("colpack", [L, 128, NCP])
        self.walaug = dr("walaug", [L, 33, 128])
        self.fw1 = dr("ffn_w1", [ND, D, DFF])
        self.fw3 = dr("ffn_w3", [ND, D, DFF])
        self.fw2 = dr("ffn_w2", [ND, DFF, D])
        self.rw = dr("router_w", [max(NM, 1), D, NE])
        self.mw1 = dr("moe_w1", [max(NM, 1), NE, D, DFF])
        self.mw3 = dr("moe_w3", [max(NM, 1), NE, D, DFF])
        self.mw2 = dr("moe_w2", [max(NM, 1), NE, DFF, D])
        self.fing = dr("fing", [128, D])
        self.c32 = dr("c32", [128, 5 * 128 + 8 + 256])
        self.identd = dr("identd", [128, 128], BF16)
        self.cosd = dr("cosT", [128, S])
        self.sind = dr("sinT", [128, S])
        self.out = dr("out", [S, D], F32, "ExternalOutput")
        self.xres = dr("xres", [S, D], F32, "Internal")
        self.hT_d = dr("hT_d", [8, 128, S], BF16, "Internal")
        self.yT_d = dr("yT_d", [8, 128, S], BF16, "Internal")
        self.hT2_d = dr("hT2_d", [8, 128, S], BF16, "Internal")

        with ExitStack() as st0:
            self.P = Prog(nc, st0)
            P = self.P
            self.pf = [Buf(st0.enter_context(nc.psum_tensor("pf%d" % i, [128, 512], F32)), True) for i in range(6)]
            self.pb = [Buf(st0.enter_context(nc.psum_tensor("pb%d" % i, [128, 1024], BF16)), True) for i in range(2)]
            self.pf_free = list(self.pf)
            self.pb_free = list(self.pb)
            self.ident = self.sb(st0, "ident", [128, 128], BF16)
            self.cc = self.sb(st0, "cc", [128, 5 * 128 + 8 + 256], F32)
            P.dma("sp", self.ident[:], self.identd, writes=[self.ident])
            P.dma("sp", self.cc[:], self.c32, writes=[self.cc])
            cc = self.cc
            self.tri = cc[:, 0:128]
            self.neg = cc[:, 128:256]
            self.ones = cc[:, 256:384]
            self.bd01 = cc[:, 384:512]
            self.kapR = cc[:, 640:644]
            self.qdec = cc[:, 644:648]
            self.EMr = cc[:, 648:904]
            self.gate = self.sb(st0, "gate", [128, S // 128, NE], F32)
            self.xrow = [Buf(None) for _ in range(S // 128)]

            for l in range(L):
                xsrc = self.x_in if l == 0 else self.xres
                if 'A' in self.ph:
                    self.phaseA(l, xsrc)
                    P.barrier()
                if 'B' in self.ph:
                    self.phaseB(l, xsrc)
                    P.barrier()
                if 'C' in self.ph:
                    self.phaseC(l)
                    P.barrier()
            if 'D' in self.ph:
                self.phaseD()
            P.barrier()
            P.emit()
        return nc

    def phaseA(self, l, xsrc):
        nc, P, S, NT = self.nc, self.P, self.S, self.NT
        tri, neg, ones, bd01 = self.tri, self.neg, self.ones, self.bd01
        cc = self.cc
        with ExitStack() as st:
            sb = lambda name, shape, dt=F32: self.sb(st, name, shape, dt)
            wA = [sb("wA%d" % k, [128, NA], BF16) for k in range(8)]
            for k in range(8):
                P.dma("pool", wA[k][:], self.wA[l, k * 128:(k + 1) * 128, :], writes=[wA[k]])
            rr = sb("rr", [128, NRR])
            P.dma("sp", rr[:], self.rowrep[l], writes=[rr])
            cpk = sb("cpk", [128, NCP])
            P.dma("sp", cpk[:], self.colpack[l], writes=[cpk])
            wal = sb("wal", [33, 128], BF16)
            P.dma("pool", wal[:], self.walaug[l], writes=[wal])
            arep = sb("arep", [128, 4])
            self.act(arep[:], rr[:, RR_ALOG:RR_ALOG + 4], AF.Exp, [rr], [arep])
            self.ts("dve", arep[:], arep[:], -1.0, None, ALU.mult, None, [arep], [arep])

            xb = [sb("xb%d" % i, [128, D]) for i in range(2)]
            hb = sb("hb", [128, D], BF16)
            hT = sb("hT", [128, 8, 512], BF16)
            cosb = sb("cosb", [128, 512])
            sinb = sb("sinb", [128, 512])
            rtmp = [sb("rtmp%d" % i, [128, 512]) for i in range(2)]
            rqT = sb("rqT", [128, 2, 512], BF16)
            rkT = sb("rkT", [128, 2, 512], BF16)
            raw = [sb("raw%d" % i, [128, 515]) for i in range(2)]
            halo = sb("halo", [128, 10, 3])
            cacc = [sb("cacc%d" % i, [128, 512]) for i in range(2)]
            cvT = sb("cvT", [128, 10, 512], BF16)
            gaT = sb("gaT", [33, 512], BF16)
            st4 = sb("st4", [128, 12])
            smN = sb("smN", [128, 8])
            smR = sb("smR", [128, 16])
            smS = sb("smS", [128, 64])
            smG = sb("smG", [128, 16])
            smM = sb("smM", [128, 64])
            vr = sb("vr", [128, 4, 64], BF16)
            sgr = sb("sgr", [128, 256])
            e1 = sb("e1", [128, 128])
            lap = sb("lap", [128, 4, 64])
            eq = sb("eq", [128, 4, 64])
            ek = sb("ek", [128, 4, 64])
            qpad = sb("qpad", [128, 4, 64], BF16)
            kpad = sb("kpad", [128, 4, 64], BF16)
            qkTg = sb("qkTg", [128, 4, 128], BF16)
            vg = sb("vg", [128, 4, 64], BF16)
            vm = sb("vm", [128, 4, 65], BF16)
            sgm = sb("sgm", [128, 256])
            sgz = sb("sgz", [128, 512])
            ktr = sb("ktr", [128, 256], BF16)
            ktm = sb("ktm", [128, 256], BF16)
            xBt = sb("xBt", [128, 4, 128], BF16)
            Rb = sb("Rb", [128, 4, 128])
            Dm = sb("Dm", [128, 4, 128])
            pTs_ = {m: sb("pT" + m, [128, 4, 128], BF16) for m in "RSGM"}
            vs = sb("vs", [128, 4, 64], BF16)
            vs2 = sb("vs2", [128, 4, 64], BF16)
            ob = {m: [sb("o%s%d" % (m, i), [128, 256]) for i in range(3)] for m in "RSGM"}
            gsgs = {m: sb("gsg" + m, [128, 256]) for m in "RGM"}
            ytok = {m: sb("ytok" + m, [128, 256], BF16) for m in "RSGM"}
            yT = sb("yT", [128, 8, 512], BF16)
            Sr = sb("Sr", [128, 2, 128]); Sr16 = sb("Sr16", [128, 2, 128], BF16)
            Sg = sb("Sg", [128, 2, 128]); Sg16 = sb("Sg16", [128, 2, 128], BF16)
            Sm = sb("Sm", [128, 2, 130]); Sm16 = sb("Sm16", [128, 2, 130], BF16)
            Ss = sb("Ss", [128, 4, 64]); Ss16 = sb("Ss16", [128, 4, 64], BF16)
            EMg = sb("EMg", [128, 2, 128])
            EMm = sb("EMm", [128, 2, 130])
            for b_ in (Sr, Sg, Sm, Ss, EMm, lap, halo):
                self.ms("dve", b_[:], 0.0, [b_])
            for b_ in (Sr16, Sg16, Sm16, Ss16, qpad, kpad, gaT):
                self.ms("pool", b_[:], 0.0, [b_])
            self.ms("pool", gaT[32:33, :], 1.0, [gaT])

            def rstd(out, in_, scale, buf):
                self.act(out, in_, AF.Ln, [buf], [buf], scale=scale, bias=EPS)
                self.act(out, out, AF.Exp, [buf], [buf], scale=-0.5)

            def sigm(out, x_ap, xb_, ob_):
                self.act(out, x_ap, AF.Exp, [xb_], [ob_], scale=-1.0)
                self.act(out, out, AF.Ln, [ob_], [ob_], bias=1.0)
                self.act(out, out, AF.Exp, [ob_], [ob_], scale=-1.0)

            v4 = lambda ap: ap.rearrange("p (h v) -> p h v", h=4)

            for t in range(NT):
                tsl = slice(t * 512, (t + 1) * 512)
                for c in range(4):
                    row0 = (t * 4 + c) * 128
                    xt = xb[c % 2]
                    P.dma("sp", xt[:], xsrc[row0:row0 + 128, :], writes=[xt])
                    self.act(hb[:], xt[:], AF.Square, [xt], [hb, smN], accum=smN[:, 0:1])
                    rstd(smN[:, 1:2], smN[:, 0:1], 1.0 / D, smN)
                    self.stt("dve", hb[:], xt[:], smN[:, 1:2], rr[:, RR_MIXG:RR_MIXG + D], ALU.mult, ALU.mult, [xt, smN, rr], [hb])
                    pt = self.psb()
                    for k in range(8):
                        self.tr(pt[:, k * 128:(k + 1) * 128], hb[:, k * 128:(k + 1) * 128], [hb], [pt])
                    self.cp("act", hT[:, :, c * 128:(c + 1) * 128], pt[:].rearrange("p (k n) -> p k n", k=8), [pt], [hT])
                    self.prel(pt)
                P.dma("pool", self.hT_d[:, :, tsl].rearrange("k p n -> p k n"), hT[:], reads=[hT])
                P.dma("sp", cosb[:], self.cosd[:, tsl], writes=[cosb])
                P.dma("sp", sinb[:], self.sind[:, tsl], writes=[sinb])

                def fm(b, M=128):
                    ps = self.psf()
                    for k in range(8):
                        self.mm(ps[0:M, :], wA[k][:, b * 128:b * 128 + M], hT[:, k, :], k == 0, k == 7, [wA[k], hT], [ps])
                    return ps
                for qk, dst in ((0, rqT), (1, rkT)):
                    for p in range(2):
                        ps1 = fm(qk * 4 + p)
                        self.tt("dve", rtmp[0][:], ps1[:], cosb[:], ALU.mult, [ps1, cosb], [rtmp[0]])
                        self.prel(ps1)
                        ps2 = fm(qk * 4 + 2 + p)
                        self.tt("dve", rtmp[1][:], ps2[:], sinb[:], ALU.mult, [ps2, sinb], [rtmp[1]])
                        self.prel(ps2)
                        self.tt("pool", dst[:, p, :], rtmp[0][:], rtmp[1][:], ALU.add, [rtmp[0], rtmp[1]], [dst])
                ps = fm(18, M=16)
                self.cp("act", gaT[0:16, :], ps[0:16, :], [ps], [gaT])
                self.prel(ps)
                for b in range(10):
                    ps = fm(8 + b)
                    rw_ = raw[b % 2]
                    ac = cacc[b % 2]
                    self.cp("act", rw_[:, 3:515], ps[:], [ps], [rw_])
                    self.prel(ps)
                    self.cp("pool", rw_[:, 0:3], halo[:, b, :], [halo], [rw_])
                    cw = lambda k_: cpk[:, CP_CW + b * 4 + k_:CP_CW + b * 4 + k_ + 1]
                    self.ts("dve", ac[:], rw_[:, 0:512], cw(0), cpk[:, CP_CB + b:CP_CB + b + 1], ALU.mult, ALU.add, [rw_, cpk], [ac])
                    for k_ in range(1, 4):
                        self.stt("dve", ac[:], rw_[:, k_:k_ + 512], cw(k_), ac[:], ALU.mult, ALU.add, [rw_, cpk, ac], [ac])
                    self.cp("pool", halo[:, b, :], rw_[:, 512:515], [rw_], [halo])
                    self.act(cvT[:, b, :], ac[:], AF.Silu, [ac], [cvT])

                for c in range(4):
                    csl = slice(c * 128, (c + 1) * 128)

                    def tmproj(j, ncol):
                        ps = self.psf()
                        off = TMO + j * 512
                        for k in range(8):
                            self.mm(ps[:, 0:ncol], hT[:, k, csl], wA[k][:, off:off + ncol], k == 0, k == 7, [hT, wA[k]], [ps])
                        return ps

                    ps = tmproj(4, 12)
                    self.cp("act", st4[:], ps[:, 0:12], [ps], [st4])
                    self.prel(ps)
                    ps3 = tmproj(3, 512)
                    sigm(sgz[:], ps3[:], ps3, sgz)
                    self.tt("dve", sgz[:], sgz[:], ps3[:], ALU.mult, [sgz, ps3], [sgz])
                    self.prel(ps3)

                    def sec_ret():
                        sm = smR
                        o1, o2, o3 = ob["R"]
                        gsg = gsgs["R"]
                        yield from self.wf('f')
                        ps = tmproj(0, 512)
                        self.tt("dve", vr[:], v4(ps[:, 0:256]), bc(self.kapR, [128, 4, 64], 2), ALU.mult, [ps, cc], [vr])
                        yield
                        sigm(sgr[:], ps[:, 256:512], ps, sgr)
                        yield
                        self.tt("dve", sgr[:], sgr[:], ps[:, 256:512], ALU.mult, [sgr, ps], [sgr])
                        self.prel(ps)
                        self.tt("pool", gsg[:], sgr[:], rr[:, RR_RETG:RR_RETG + 256], ALU.mult, [sgr, rr], [gsg])
                        yield
                        yield from self.wf('b')
                        ptb = self.psb()
                        for p in range(2):
                            self.tr(ptb[:, p * 128:(p + 1) * 128], rkT[:, p, csl], [rkT], [ptb])
                        self.cp("act", ktr[:], ptb[:, 0:256], [ptb], [ktr])
                        self.prel(ptb)
                        yield
                        yield from self.lin_core(rqT, rkT, csl, ktr, vr, 64, Sr, Sr16, self.EMr.rearrange("p (a b) -> p a b", a=2), [cc], pTs_["R"])
                        pso = self.last_pso["R"] if False else self._pso
                        self.tt("dve", v4(o1[:]), v4(pso[:, 0:256]), bc(self.qdec, [128, 4, 64], 2), ALU.mult, [pso, cc], [o1])
                        self.prel(pso)
                        yield
                        yield from self.head_norm(o1, o2, o3, sm, gsg, ytok["R"])

                    def sec_ssd():
                        sm = smS
                        o1, o2, o3 = ob["S"]
                        self.tt("dve", sm[:, 8:12], st4[:, 0:4], rr[:, RR_DTB:RR_DTB + 4], ALU.add, [st4, rr], [sm])
                        self.act(sm[:, 12:16], sm[:, 8:12], AF.Exp, [sm], [sm])
                        yield
                        self.act(sm[:, 16:20], sm[:, 12:16], AF.Ln, [sm], [sm], bias=1.0)
                        yield
                        self.tt("dve", sm[:, 20:24], sm[:, 16:20], arep[:], ALU.mult, [sm, arep], [sm])
                        yield
                        yield from self.wf('f')
                        psA = self.psf()
                        self.mm(psA[:, 0:4], tri, sm[:, 20:24], True, True, [cc, sm], [psA])
                        self.mm(psA[:, 4:8], ones, sm[:, 20:24], True, True, [cc, sm], [psA])
                        self.tt("dve", Rb[:], bc(tri, [128, 4, 128], 1), bc(sm[:, 20:24], [128, 4, 128], 2), ALU.mult, [cc, sm], [Rb])
                        yield
                        self.cp("act", sm[:, 24:32], psA[:, 0:8], [psA], [sm])
                        self.prel(psA)
                        yield from self.wf('f')
                        psR = self.psf()
                        self.mm(psR[:], ones, Rb[:].rearrange("p h i -> p (h i)"), True, True, [cc, Rb], [psR])
                        yield
                        self.tt("dve", Dm[:], psR[:].rearrange("p (h i) -> p h i", h=4), bc(sm[:, 24:28], [128, 4, 128], 2), ALU.subtract, [psR, sm], [Dm])
                        self.prel(psR)
                        yield
                        self.tt("pool", Dm[:], Dm[:], bc(neg, [128, 4, 128], 1), ALU.add, [Dm, cc], [Dm])
                        yield
                        self.act(Dm[:], Dm[:], AF.Exp, [Dm], [Dm])
                        yield from self.wf('b')
                        ptb = self.psb()
                        for j in range(4):
                            self.tr(ptb[:, j * 128:(j + 1) * 128], cvT[:, j, csl], [cvT], [ptb])
                        self.cp("act", xBt[:], ptb[:, 0:512].rearrange("p (a n) -> p a n", a=4), [ptb], [xBt])
                        self.prel(ptb)
                        yield
                        xtok = xBt[:, 0:2, :].rearrange("p a (r v) -> p (a r) v", r=2)
                        self.tt("dve", vs[:], xtok, bc(sm[:, 16:20], [128, 4, 64], 2), ALU.mult, [xBt, sm], [vs])
                        self.tt("dve", sm[:, 32:36], sm[:, 28:32], sm[:, 24:28], ALU.subtract, [sm], [sm])
                        yield
                        self.act(sm[:, 36:40], sm[:, 32:36], AF.Exp, [sm], [sm])
                        self.act(sm[:, 40:48], sm[:, 24:32], AF.Exp, [sm], [sm])
                        yield from self.wf('f')
                        pss = self.psf()
                        for g in range(2):
                            self.mm(pss[:, g * 128:(g + 1) * 128], cvT[:, 2 + g, csl], cvT[:, 4 + g, csl], True, True, [cvT], [pss])
                        yield
                        self.tt("dve", vs2[:], vs[:], bc(sm[:, 36:40], [128, 4, 64], 2), ALU.mult, [vs, sm], [vs2])
                        pTs = pTs_["S"]
                        for g in range(2):
                            self.tt("dve", pTs[:, 2 * g:2 * g + 2, :], bc(pss[:, g * 128:(g + 1) * 128], [128, 2, 128], 1), Dm[:, 2 * g:2 * g + 2, :], ALU.mult, [pss, Dm], [pTs])
                        self.prel(pss)
                        yield
                        yield from self.wf('f', 2)
                        psy = self.psf()
                        for h in range(4):
                            self.mm(psy[:, h * 64:(h + 1) * 64], pTs[:, h, :], vs[:, h, :], True, True, [pTs, vs], [psy])
                        for g in range(2):
                            self.mm(psy[:, 256 + g * 128:256 + (g + 1) * 128], cvT[:, 4 + g, csl], Ss16[:, 2 * g:2 * g + 2, :].rearrange("p a v -> p (a v)"), True, True, [cvT, Ss16], [psy])
                        psu = self.psf()
                        for g in range(2):
                            self.mm(psu[:, g * 128:(g + 1) * 128], xBt[:, 2 + g, :], vs2[:, 2 * g:2 * g + 2, :].rearrange("p a v -> p (a v)"), True, True, [xBt, vs2], [psu])
                        yield
                        self.tt("dve", Ss[:], Ss[:], bc(sm[:, 44:48], [128, 4, 64], 2), ALU.mult, [Ss, sm], [Ss])
                        yield
                        self.tt("dve", Ss[:], Ss[:], v4(psu[:, 0:256]), ALU.add, [Ss, psu], [Ss])
                        self.prel(psu)
                        yield
                        self.cp("act", Ss16[:], Ss[:], [Ss], [Ss16])
                        self.tt("dve", v4(o1[:]), v4(psy[:, 256:512]), bc(sm[:, 40:44], [128, 4, 64], 2), ALU.mult, [psy, sm], [o1])
                        yield
                        self.tt("dve", o1[:], o1[:], psy[:, 0:256], ALU.add, [o1, psy], [o1])
                        self.prel(psy)
                        self.tt("pool", v4(o2[:]), xtok, v4(rr[:, RR_DVEC:RR_DVEC + 256]), ALU.mult, [xBt, rr], [o2])
                        yield
                        self.tt("dve", o1[:], o1[:], o2[:], ALU.add, [o1, o2], [o1])
                        yield
                        self.tt("dve", o1[:], o1[:], sgz[:, 0:256], ALU.mult, [o1, sgz], [o1])
                        yield
                        self.act(o3[:], o1[:], AF.Square, [o1], [o3, sm], accum=sm[:, 48:49])
                        yield
                        rstd(sm[:, 49:50], sm[:, 48:49], 1.0 / 256, sm)
                        yield
                        self.stt("dve", ytok["S"][:], o1[:], sm[:, 49:50], rr[:, RR_SSDG:RR_SSDG + 256], ALU.mult, ALU.mult, [o1, sm, rr], [ytok["S"]])

                    def sec_gla():
                        sm = smG
                        o1, o2, o3 = ob["G"]
                        gsg = gsgs["G"]
                        yield from self.wf('f')
                        psa = self.psf()
                        self.mm(psa[:, 0:128], gaT[0:33, csl], wal[:], True, True, [gaT, wal], [psa])
                        self.act(e1[:], psa[:, 0:128], AF.Exp, [psa], [e1], scale=-1.0)
                        self.prel(psa)
                        yield
                        self.act(e1[:], e1[:], AF.Ln, [e1], [e1], bias=1.0)
                        yield
                        self.ts("dve", lap[:, :, 0:32], e1[:].rearrange("p (h d) -> p h d", h=4), -1.0 / 16.0, None, ALU.mult, None, [e1], [lap])
                        self.tt("pool", gsg[:], sgz[:, 256:512], rr[:, RR_GLAG:RR_GLAG + 256], ALU.mult, [sgz, rr], [gsg])
                        yield
                        yield from self.wf('f')
                        psg = self.psf()
                        self.mm(psg[:, 0:256], tri, lap[:].rearrange("p h d -> p (h d)"), True, True, [cc, lap], [psg])
                        for p in range(2):
                            self.mm(psg[:, 256 + p:257 + p], lap[:, 2 * p:2 * p + 2, :].rearrange("p a d -> p (a d)"), ones[:, 0:1], True, True, [lap, cc], [psg])
                        yield
                        self.act(eq[:], v4(psg[:, 0:256]), AF.Exp, [psg], [eq])
                        self.act(ek[:], v4(psg[:, 0:256]), AF.Exp, [psg], [ek], scale=-1.0)
                        self.act(sm[:, 0:2], psg[:, 256:258], AF.Exp, [psg], [sm])
                        self.prel(psg)
                        yield
                        for p in range(2):
                            self.ts("dve", EMg[:, p, :], bd01, sm[:, p:p + 1], None, ALU.mult, None, [cc, sm], [EMg])
                        yield from self.wf('f')
                        ps1 = tmproj(1, 512)
                        yield
                        self.stt("dve", qpad[:, :, 0:32], ps1[:, 0:128].rearrange("p (h d) -> p h d", h=4), 32.0 ** -0.5, eq[:, :, 0:32], ALU.mult, ALU.mult, [ps1, eq], [qpad])
                        yield
                        self.tt("dve", kpad[:, :, 0:32], ps1[:, 128:256].rearrange("p (h d) -> p h d", h=4), ek[:, :, 0:32], ALU.mult, [ps1, ek], [kpad])
                        yield
                        self.cp("act", vg[:], v4(ps1[:, 256:512]), [ps1], [vg])
                        self.prel(ps1)
                        yield from self.wf('b')
                        ptb = self.psb()
                        for p in range(2):
                            self.tr(ptb[:, p * 128:(p + 1) * 128], qpad[:, 2 * p:2 * p + 2, :].rearrange("p a d -> p (a d)"), [qpad], [ptb])
                            self.tr(ptb[:, 256 + p * 128:256 + (p + 1) * 128], kpad[:, 2 * p:2 * p + 2, :].rearrange("p a d -> p (a d)"), [kpad], [ptb])
                        self.cp("act", qkTg[:], ptb[:, 0:512].rearrange("p (a n) -> p a n", a=4), [ptb], [qkTg])
                        self.prel(ptb)
                        yield
                        yield from self.lin_core(qkTg, qkTg, None, kpad[:].rearrange("p h d -> p (h d)"), vg, 64, Sg, Sg16, EMg[:], [EMg], pTs_["G"], koff=2, ktok_buf=kpad)
                        pso = self._pso
                        self.cp("act", o1[:], pso[:, 0:256], [pso], [o1])
                        self.prel(pso)
                        yield
                        yield from self.head_norm(o1, o2, o3, sm, gsg, ytok["G"])

                    def sec_ml():
                        sm = smM
                        o1, o2, o3 = ob["M"]
                        gsg = gsgs["M"]
                        yield from self.wf('f')
                        ps2 = tmproj(2, 512)
                        sigm(sgm[:], ps2[:, 256:512], ps2, sgm)
                        yield
                        self.tt("pool", gsg[:], sgm[:], rr[:, RR_MLG:RR_MLG + 256], ALU.mult, [sgm, rr], [gsg])
                        self.tt("dve", sm[:, 8:12], st4[:, 8:12], rr[:, RR_BF:RR_BF + 4], ALU.add, [st4, rr], [sm])
                        yield
                        self.act(sm[:, 12:16], sm[:, 8:12], AF.Exp, [sm], [sm], scale=-1.0)
                        yield
                        self.act(sm[:, 16:20], sm[:, 12:16], AF.Ln, [sm], [sm], bias=1.0)
                        yield
                        yield from self.wf('f')
                        psb_ = self.psf()
                        self.mm(psb_[:, 0:4], tri, sm[:, 16:20], True, True, [cc, sm], [psb_])
                        self.mm(psb_[:, 4:8], ones, sm[:, 16:20], True, True, [cc, sm], [psb_])
                        self.tt("dve", sm[:, 20:24], st4[:, 4:8], rr[:, RR_BI:RR_BI + 4], ALU.add, [st4, rr], [sm])
                        yield
                        self.tt("dve", sm[:, 20:24], sm[:, 20:24], psb_[:, 0:4], ALU.add, [sm, psb_], [sm])
                        yield
                        self.act(sm[:, 24:28], sm[:, 20:24], AF.Exp, [sm], [sm], bias=math.log(0.125))
                        self.act(sm[:, 28:36], psb_[:, 0:8], AF.Exp, [psb_], [sm], scale=-1.0)
                        self.prel(psb_)
                        yield
                        self.tt("dve", vm[:, :, 0:64], v4(ps2[:, 0:256]), bc(sm[:, 24:28], [128, 4, 64], 2), ALU.mult, [ps2, sm], [vm])
                        self.prel(ps2)
                        yield
                        self.cp("dve", vm[:, :, 64:65], sm[:, 24:28].unsqueeze(2), [sm], [vm])
                        for p in range(2):
                            for r in range(2):
                                hh = 2 * p + r
                                self.ts("dve", EMm[r * 64:(r + 1) * 64, p, r * 65:(r + 1) * 65], ones[r * 64:(r + 1) * 64, 0:65], sm[r * 64:(r + 1) * 64, 32 + hh:33 + hh], None, ALU.mult, None, [cc, sm], [EMm])
                            yield
                        yield from self.wf('b')
                        ptb = self.psb()
                        for p in range(2):
                            self.tr(ptb[:, p * 128:(p + 1) * 128], cvT[:, 8 + p, csl], [cvT], [ptb])
                        self.cp("act", ktm[:], ptb[:, 0:256], [ptb], [ktm])
                        self.prel(ptb)
                        yield
                        yield from self.lin_core(cvT, cvT, csl, ktm, vm, 65, Sm, Sm16, EMm[:], [EMm], pTs_["M"], qoff=6, koff=8)
                        pso = self._pso
                        pso3 = pso[:, 0:260].rearrange("p (h v) -> p h v", h=4)
                        self.tt("dve", sm[:, 36:40], pso3[:, :, 64], sm[:, 28:32], ALU.mult, [pso, sm], [sm])
                        yield
                        self.stt("dve", sm[:, 40:44], sm[:, 36:40], -1.0, sm[:, 36:40], ALU.mult, ALU.max, [sm], [sm])
                        yield
                        self.ts("dve", sm[:, 40:44], sm[:, 40:44], 1.0, None, ALU.max, None, [sm], [sm])
                        yield
                        self.P.op("dve", lambda e: e.reciprocal(out=sm[:, 44:48], in_=sm[:, 40:44]), [sm], [sm])
                        yield
                        self.tt("dve", sm[:, 44:48], sm[:, 44:48], sm[:, 28:32], ALU.mult, [sm], [sm])
                        yield
                        self.tt("dve", v4(o1[:]), pso3[:, :, 0:64], bc(sm[:, 44:48], [128, 4, 64], 2), ALU.mult, [pso, sm], [o1])
                        self.prel(pso)
                        yield
                        yield from self.head_norm(o1, o2, o3, sm, gsg, ytok["M"])

                    gens = []
                    if 'ret' in self.en:
                        gens.append(sec_ret())
                    if 'ssd' in self.en:
                        gens.append(sec_ssd())
                    if 'gla' in self.en:
                        gens.append(sec_gla())
                    if 'ml' in self.en:
                        gens.append(sec_ml())
                    while gens:
                        for g_ in list(gens):
                            try:
                                next(g_)
                            except StopIteration:
                                gens.remove(g_)

                    ptb = self.psb()
                    for j, m in enumerate("RSGM"):
                        for vc in range(2):
                            self.tr(ptb[:, (2 * j + vc) * 128:(2 * j + vc + 1) * 128], ytok[m][:, vc * 128:(vc + 1) * 128], [ytok[m]], [ptb])
                    self.cp("act", yT[:, :, csl], ptb[:].rearrange("p (k n) -> p k n", k=8), [ptb], [yT])
                    self.prel(ptb)
                P.dma("pool", self.yT_d[:, :, tsl].rearrange("k p n -> p k n"), yT[:], reads=[yT])

    def lin_core(self, qT, kT, csl, ktok, v, dv, S_, S16, EM, EMb, pTb, qoff=0, koff=0, ktok_buf=None):
        tri = self.tri
        cc = self.cc
        if csl is None:
            qa = lambda p, r: qT[r * 64:(r + 1) * 64, qoff + p, :]
            ka = lambda p, r: kT[r * 64:(r + 1) * 64, koff + p, :]
        else:
            qa = lambda p, r: qT[r * 64:(r + 1) * 64, qoff + p, csl]
            ka = lambda p, r: kT[r * 64:(r + 1) * 64, koff + p, csl]
        yield from self.wf('f', 2)
        pssr = [self.psf(), self.psf()]
        for h in range(4):
            p, r = h // 2, h % 2
            self.mm(pssr[r][:, p * 128:(p + 1) * 128], ka(p, r), qa(p, r), True, True, [kT, qT], [pssr[r]])
        yield
        for r in range(2):
            self.tt("dve", pTb[:, r:4:2, :], pssr[r][:, 0:256].rearrange("p (a i) -> p a i", a=2), bc(tri, [128, 2, 128], 1), ALU.mult, [pssr[r], cc], [pTb])
            self.prel(pssr[r])
            yield
        yield from self.wf('f', 2)
        pso = self.psf()
        for h in range(4):
            p, r = h // 2, h % 2
            self.mm(pso[:, h * dv:(h + 1) * dv], pTb[:, h, :], v[:, h, :], True, False, [pTb, v], [pso])
            self.mm(pso[:, h * dv:(h + 1) * dv], qa(p, r), S16[r * 64:(r + 1) * 64, p, r * dv:(r + 1) * dv], False, True, [qT, S16], [pso])
        psu = self.psf()
        kb = ktok_buf if ktok_buf is not None else ktok
        for p in range(2):
            self.mm(psu[:, p * 2 * dv:(p + 1) * 2 * dv], ktok[:, p * 128:(p + 1) * 128], v[:, 2 * p:2 * p + 2, :].rearrange("p a v -> p (a v)"), True, True, [kb, v], [psu])
        yield
        self.tt("dve", S_[:], S_[:], psu[:, 0:4 * dv].rearrange("p (a v) -> p a v", a=2), ALU.add, [S_, psu], [S_])
        self.prel(psu)
        yield
        self.tt("dve", S_[:], S_[:], EM, ALU.mult, [S_] + list(EMb), [S_])
        yield
        self.cp("act", S16[:], S_[:], [S_], [S16])
        self._pso = pso

    def head_norm(self, o1, o2, o3, sm, gsg, yout):
        self.act(o2[:], o1[:], AF.Square, [o1], [o2])
        yield
        self.P.op("dve", lambda e: e.tensor_reduce(out=sm[:, 4:8], in_=o2[:].rearrange("p (h v) -> p h v", h=4), axis=AX.X, op=ALU.add), [o2], [sm])
        yield
        self.act(sm[:, 4:8], sm[:, 4:8], AF.Ln, [sm], [sm], scale=1.0 / 64, bias=EPS)
        yield
        self.act(sm[:, 4:8], sm[:, 4:8], AF.Exp, [sm], [sm], scale=-0.5)
        yield
        self.tt("dve", o3[:].rearrange("p (h v) -> p h v", h=4), o1[:].rearrange("p (h v) -> p h v", h=4), bc(sm[:, 4:8], [128, 4, 64], 2), ALU.mult, [o1, sm], [o3])
        yield
        self.tt("dve", yout[:], o3[:], gsg[:], ALU.mult, [o3, gsg], [yout])


    def phaseB(self, l, xsrc):
        nc, P, S, NT = self.nc, self.P, self.S, self.NT
        moe = (l % 2 == 1)
        with ExitStack() as st:
            sb = lambda name, shape, dt=F32: self.sb(st, name, shape, dt)
            wG = [sb("wG%d" % k, [128, 4096], BF16) for k in range(8)]
            for k in range(8):
                P.dma("pool", wG[k][:], self.w_in[l, k * 128:(k + 1) * 128, 3868:7964], writes=[wG[k]])
            wb = [sb("wb%d" % j, [128, D], BF16) for j in range(8)]
            for j in range(8):
                n_, vc = j // 2, j % 2
                P.dma("pool", wb[j][:], self.wbr[l, n_, vc * 128:(vc + 1) * 128, :], writes=[wb[j]])
            wo = [sb("wo%d" % k, [128, D], BF16) for k in range(8)]
            for k in range(8):
                P.dma("pool", wo[k][:], self.wout[l, k * 128:(k + 1) * 128, :], writes=[wo[k]])
            gr = sb("grB", [128, D])
            P.dma("sp", gr[:], self.rowrep[l, :, RR_FFNG:RR_FFNG + D], writes=[gr])
            if moe:
                wr = sb("wr", [128, 8, NE], BF16)
                P.dma("pool", wr[:], self.rw[l // 2].rearrange("(k p) e -> p k e", p=128), writes=[wr])
            hTs = [sb("hTB%d" % i, [128, 8, 512], BF16) for i in range(2)]
            yTs = [sb("yTB%d" % i, [128, 8, 512], BF16) for i in range(2)]
            mg = sb("mg", [128, 8, 512], BF16)
            sg = [sb("sg%d" % i, [128, 512]) for i in range(2)]
            macc = sb("macc", [128, 512])
            xt = [sb("xtB%d" % i, [128, D]) for i in range(2)]
            sqj = sb("sqjB", [128, D], BF16)
            hb = sb("hbB", [128, D], BF16)
            hT2 = sb("hT2B", [128, 8, 512], BF16)
            sm = sb("smB", [128, 64])
            lg = sb("lg", [128, 4, NE])
            for t in range(NT):
                tsl = slice(t * 512, (t + 1) * 512)
                hT = hTs[t % 2]
                yT = yTs[t % 2]
                P.dma("sp", hT[:], self.hT_d[:, :, tsl].rearrange("k p n -> p k n"), writes=[hT])
                P.dma("sp", yT[:], self.yT_d[:, :, tsl].rearrange("k p n -> p k n"), writes=[yT])
                for dc in range(8):
                    for n_ in range(4):
                        psg = self.psf()
                        for k in range(8):
                            self.mm(psg[:], wG[k][:, n_ * D + dc * 128:n_ * D + (dc + 1) * 128], hT[:, k, :], k == 0, k == 7, [wG[k], hT], [psg])
                        psb_ = self.psf()
                        for vc in range(2):
                            self.mm(psb_[:], wb[n_ * 2 + vc][:, dc * 128:(dc + 1) * 128], yT[:, n_ * 2 + vc, :], vc == 0, vc == 1, [wb[n_ * 2 + vc], yT], [psb_])
                        s_ = sg[n_ % 2]
                        self.act(s_[:], psg[:], AF.Sigmoid, [psg], [s_])
                        self.prel(psg)
                        if n_ == 0:
                            self.tt("dve", macc[:], s_[:], psb_[:], ALU.mult, [s_, psb_], [macc])
                            self.prel(psb_)
                        else:
                            self.tt("dve", s_[:], s_[:], psb_[:], ALU.mult, [s_, psb_], [s_])
                            self.prel(psb_)
                            if n_ < 3:
                                self.tt("pool", macc[:], macc[:], s_[:], ALU.add, [macc, s_], [macc])
                            else:
                                self.tt("dve", mg[:, dc, :], macc[:], s_[:], ALU.add, [macc, s_], [mg])
                for c in range(4):
                    row0 = (t * 4 + c) * 128
                    csl = slice(c * 128, (c + 1) * 128)
                    x_ = xt[c % 2]
                    P.dma("sp", x_[:], xsrc[row0:row0 + 128, :], writes=[x_])
                    for hf in range(2):
                        ps = self.psf()
                        for k in range(8):
                            self.mm(ps[:], mg[:, k, csl], wo[k][:, hf * 512:(hf + 1) * 512], k == 0, k == 7, [mg, wo[k]], [ps])
                        self.tt("dve", x_[:, hf * 512:(hf + 1) * 512], x_[:, hf * 512:(hf + 1) * 512], ps[:], ALU.add, [x_, ps], [x_])
                        self.prel(ps)
                    P.dma("pool", self.xres[row0:row0 + 128, :], x_[:], reads=[x_], writes=[self.xrow[t * 4 + c]])
                    self.act(sqj[:], x_[:], AF.Square, [x_], [sqj, sm], accum=sm[:, 0:1])
                    self.act(sm[:, 1:2], sm[:, 0:1], AF.Sqrt, [sm], [sm], scale=1.0 / D, bias=EPS)
                    self.P.op("dve", lambda e: e.reciprocal(out=sm[:, 2:3], in_=sm[:, 1:2]), [sm], [sm])
                    self.stt("dve", hb[:], x_[:], sm[:, 2:3], gr[:], ALU.mult, ALU.mult, [x_, sm, gr], [hb])
                    pt = self.psb()
                    for k in range(8):
                        self.tr(pt[:, k * 128:(k + 1) * 128], hb[:, k * 128:(k + 1) * 128], [hb], [pt])
                    self.cp("act", hT2[:, :, csl], pt[:].rearrange("p (k n) -> p k n", k=8), [pt], [hT2])
                    self.prel(pt)
                    if moe:
                        ps = self.psf()
                        for k in range(8):
                            self.mm(ps[:, 0:NE], hT2[:, k, csl], wr[:, k, :], k == 0, k == 7, [hT2, wr], [ps])
                        self.cp("act", lg[:, c, :], ps[:, 0:NE], [ps], [lg])
                        self.prel(ps)
                P.dma("pool", self.hT2_d[:, :, tsl].rearrange("k p n -> p k n"), hT2[:], reads=[hT2])
                if moe:
                    self.top2(lg, sm, self.gate[:, t * 4:(t + 1) * 4, :], st, t)

    def top2(self, lg, sm, gout, st, t):
        if t == 0:
            self.t2 = [self.sb(st, "t2_%d" % i, [128, 4, NE], F32) for i in range(4)]
            self.t2s = self.sb(st, "t2s", [128, 16], F32)
        m1b, l2, m2b, tmp = self.t2
        s_ = self.t2s
        gate = self.gate
        red = lambda out, in_: self.P.op("dve", lambda e: e.tensor_reduce(out=out, in_=in_, axis=AX.X, op=ALU.max), [lg, l2], [s_])
        red(s_[:, 0:4], lg[:])
        self.tt("dve", m1b[:], lg[:], bc(s_[:, 0:4], [128, 4, NE], 2), ALU.is_equal, [lg, s_], [m1b])
        self.stt("dve", l2[:], m1b[:], -1e30, lg[:], ALU.mult, ALU.add, [m1b, lg], [l2])
        red(s_[:, 4:8], l2[:])
        self.tt("dve", m2b[:], l2[:], bc(s_[:, 4:8], [128, 4, NE], 2), ALU.is_equal, [l2, s_], [m2b])
        self.tt("dve", s_[:, 8:12], s_[:, 4:8], s_[:, 0:4], ALU.subtract, [s_], [s_])
        self.act(s_[:, 8:12], s_[:, 8:12], AF.Exp, [s_], [s_])
        self.ts("dve", s_[:, 8:12], s_[:, 8:12], 1.0, None, ALU.add, None, [s_], [s_])
        self.P.op("dve", lambda e: e.reciprocal(out=s_[:, 12:16], in_=s_[:, 8:12]), [s_], [s_])
        self.ts("dve", s_[:, 8:12], s_[:, 12:16], -1.0, 1.0, ALU.mult, ALU.add, [s_], [s_])
        self.tt("dve", m1b[:], m1b[:], bc(s_[:, 12:16], [128, 4, NE], 2), ALU.mult, [m1b, s_], [m1b])
        self.tt("dve", m2b[:], m2b[:], bc(s_[:, 8:12], [128, 4, NE], 2), ALU.mult, [m2b, s_], [m2b])
        self.tt("dve", gout, m1b[:], m2b[:], ALU.add, [m1b, m2b], [gate])

    def phaseC(self, l):
        nc, P, S, NT = self.nc, self.P, self.S, self.NT
        moe = (l % 2 == 1)
        li = l // 2
        if moe:
            items = [(e, hf) for e in range(NE) for hf in range(2)]
        else:
            items = [(None, 0), (None, 1)]
        HF = DFF // 2
        with ExitStack() as st:
            sb = lambda name, shape, dt=F32: self.sb(st, name, shape, dt)
            W1 = [[sb("W1_%d_%d" % (i, k), [128, HF], BF16) for k in range(8)] for i in range(2)]
            W3 = [[sb("W3_%d_%d" % (i, k), [128, HF], BF16) for k in range(8)] for i in range(2)]
            W2 = [[sb("W2_%d_%d" % (i, f), [128, D], BF16) for f in range(11)] for i in range(2)]
            hT2 = [sb("hT2C%d" % i, [128, 8, 512], BF16) for i in range(2)]
            h1T = sb("h1T", [128, 11, 512], BF16)
            gs = [sb("gs%d" % i, [128, 512]) for i in range(2)]
            xt = [sb("xtC%d" % i, [128, D]) for i in range(6)]
            xi = 0
            hi = 0

            def load_w(idx, slot):
                e, hf = items[idx]
                if e is None:
                    s1, s3, s2 = self.fw1[li], self.fw3[li], self.fw2[li]
                else:
                    s1, s3, s2 = self.mw1[li, e], self.mw3[li, e], self.mw2[li, e]
                for k in range(8):
                    P.dma("pool", W1[slot][k][:], s1[k * 128:(k + 1) * 128, hf * HF:(hf + 1) * HF], writes=[W1[slot][k]])
                    P.dma("pool", W3[slot][k][:], s3[k * 128:(k + 1) * 128, hf * HF:(hf + 1) * HF], writes=[W3[slot][k]])
                for f in range(11):
                    P.dma("pool", W2[slot][f][:], s2[hf * HF + f * 128:hf * HF + (f + 1) * 128, :], writes=[W2[slot][f]])

            load_w(0, 0)
            for idx, (e, hf) in enumerate(items):
                slot = idx % 2
                if idx + 1 < len(items):
                    load_w(idx + 1, (idx + 1) % 2)
                for t in range(NT):
                    tsl = slice(t * 512, (t + 1) * 512)
                    h_ = hT2[hi % 2]; hi += 1
                    P.dma("sp", h_[:], self.hT2_d[:, :, tsl].rearrange("k p n -> p k n"), writes=[h_])
                    for f in range(11):
                        pa = self.psf()
                        for k in range(8):
                            self.mm(pa[:], W1[slot][k][:, f * 128:(f + 1) * 128], h_[:, k, :], k == 0, k == 7, [W1[slot][k], h_], [pa])
                        pb_ = self.psf()
                        for k in range(8):
                            self.mm(pb_[:], W3[slot][k][:, f * 128:(f + 1) * 128], h_[:, k, :], k == 0, k == 7, [W3[slot][k], h_], [pb_])
                        g_ = gs[f % 2]
                        self.act(g_[:], pa[:], AF.Silu, [pa], [g_])
                        self.tt("dve", h1T[:, f, :], g_[:], pb_[:], ALU.mult, [g_, pb_], [h1T])
                        self.prel(pa, pb_)
                    for c in range(4):
                        row0 = (t * 4 + c) * 128
                        x_ = xt[xi % 6]; xi += 1
                        P.dma("sp", x_[:], self.xres[row0:row0 + 128, :], reads=[self.xrow[t * 4 + c]], writes=[x_])
                        for hh in range(2):
                            ps = self.psf()
                            for f in range(11):
                                self.mm(ps[:], h1T[:, f, c * 128:(c + 1) * 128], W2[slot][f][:, hh * 512:(hh + 1) * 512], f == 0, f == 10, [h1T, W2[slot][f]], [ps])
                            if e is None:
                                self.tt("dve", x_[:, hh * 512:(hh + 1) * 512], x_[:, hh * 512:(hh + 1) * 512], ps[:], ALU.add, [x_, ps], [x_])
                            else:
                                self.stt("dve", x_[:, hh * 512:(hh + 1) * 512], ps[:], self.gate[:, t * 4 + c, e:e + 1], x_[:, hh * 512:(hh + 1) * 512], ALU.mult, ALU.add, [ps, self.gate, x_], [x_])
                            self.prel(ps)
                        P.dma("pool", self.xres[row0:row0 + 128, :], x_[:], reads=[x_], writes=[self.xrow[t * 4 + c]])

    def phaseD(self):
        P, S = self.P, self.S
        with ExitStack() as st:
            sb = lambda name, shape, dt=F32: self.sb(st, name, shape, dt)
            g = sb("gD", [128, D])
            P.dma("sp", g[:], self.fing, writes=[g])
            xt = [sb("xtD%d" % i, [128, D]) for i in range(2)]
            yo = [sb("yoD%d" % i, [128, D]) for i in range(2)]
            sqj = sb("sqjD", [128, D], BF16)
            sm = sb("smD", [128, 8])
            for c in range(S // 128):
                x_ = xt[c % 2]
                y_ = yo[c % 2]
                P.dma("sp", x_[:], self.xres[c * 128:(c + 1) * 128, :], writes=[x_])
                self.act(sqj[:], x_[:], AF.Square, [x_], [sqj, sm], accum=sm[:, 0:1])
                self.act(sm[:, 1:2], sm[:, 0:1], AF.Sqrt, [sm], [sm], scale=1.0 / D, bias=EPS)
                self.P.op("dve", lambda e: e.reciprocal(out=sm[:, 2:3], in_=sm[:, 1:2]), [sm], [sm])
                self.stt("dve", y_[:], x_[:], sm[:, 2:3], g[:], ALU.mult, ALU.mult, [x_, sm, g], [y_])
                P.dma("pool", self.out[c * 128:(c + 1) * 128, :], y_[:], reads=[y_])


def _consts(S):
    j = np.arange(128)
    tri = (j[:, None] <= j[None, :]).astype(np.float32)
    neg = np.where(j[:, None] <= j[None, :], 0.0, -1e30).astype(np.float32)
    ones = np.ones((128, 128), np.float32)
    bd = ((j[:, None] // 64) == (j[None, :] // 64)).astype(np.float32)
    lg = np.log1p(-np.exp2(-5.0 - np.arange(4, dtype=np.float64)))
    kap = (np.exp(-(j[:, None] + 1.0) * lg[None, :]) * 0.125).astype(np.float32)
    qd = np.exp((j[:, None] + 1.0) * lg[None, :]).astype(np.float32)
    em = np.zeros((128, 2, 128), np.float32)
    for p in range(2):
        for r in range(2):
            em[r * 64:(r + 1) * 64, p, r * 64:(r + 1) * 64] = np.exp(128.0 * lg[2 * p + r])
    c32 = np.concatenate([tri, neg, ones, bd, np.zeros((128, 128), np.float32), kap, qd, em.reshape(128, 256)], axis=1)
    half = 32
    inv = (10000.0 ** (-np.arange(half, dtype=np.float32) / half)).astype(np.float32)
    ang = np.arange(S, dtype=np.float32)[None, :] * inv[:, None]
    cos = np.cos(ang).astype(np.float32)
    sin = np.sin(ang).astype(np.float32)
    cosT = np.concatenate([cos, cos, cos, cos], axis=0)
    sinT = np.concatenate([-sin, sin, -sin, sin], axis=0)
    ident = np.eye(128, dtype=np.float32).astype(ml_dtypes.bfloat16)
    return dict(c32=np.ascontiguousarray(c32), cosT=np.ascontiguousarray(cosT), sinT=np.ascontiguousarray(sinT), identd=ident)


def _prep_weights(inp, L):
    w_in = inp["w_in"][:L]
    o = np.cumsum([0, 256, 256, 256, 256, 256, 768, 4, 128, 128, 256, 256, 16, 512, 256, 256, 4, 4, 4096])
    (RQ, RK, RV, RG, SZ, SX, SDT, GQ, GK, GV, GR, GA, MQK, MV, MO, MI, MF, MG) = o[:18]

    def swap(c0):
        idx = []
        for h in range(4):
            idx += list(range(c0 + h * 64 + 32, c0 + h * 64 + 64)) + list(range(c0 + h * 64, c0 + h * 64 + 32))
        return idx
    cols = (list(range(RQ, RQ + 256)) + swap(RQ) + list(range(RK, RK + 256)) + swap(RK)
            + list(range(SX, SX + 768)) + list(range(MQK, MQK + 512)) + list(range(GA, GA + 16))
            + list(range(RV, RV + 256)) + list(range(RG, RG + 256))
            + list(range(GQ, GQ + 128)) + list(range(GK, GK + 128)) + list(range(GV, GV + 256))
            + list(range(MV, MV + 256)) + list(range(MO, MO + 256))
            + list(range(SZ, SZ + 256)) + list(range(GR, GR + 256))
            + list(range(SDT, SDT + 4)) + list(range(MI, MI + 4)) + list(range(MF, MF + 4)))
    assert len(cols) == NA
    wA = np.ascontiguousarray(w_in[:, :, cols])
    rowrep = np.zeros((L, NRR), np.float32)
    rowrep[:, RR_MIXG:RR_MIXG + D] = inp["mix_norm_g"][:L]
    rowrep[:, RR_FFNG:RR_FFNG + D] = inp["ffn_norm_g"][:L]
    rowrep[:, RR_RETG:RR_RETG + 256] = inp["ret_norm_g"][:L]
    rowrep[:, RR_SSDG:RR_SSDG + 256] = inp["ssd_norm_g"][:L]
    rowrep[:, RR_GLAG:RR_GLAG + 256] = inp["gla_norm_g"][:L]
    rowrep[:, RR_MLG:RR_MLG + 256] = inp["ml_norm_g"][:L]
    rowrep[:, RR_DVEC:RR_DVEC + 256] = np.repeat(inp["ssd_d"][:L], 64, axis=1)
    rowrep[:, RR_DTB:RR_DTB + 4] = inp["ssd_dt_bias"][:L]
    rowrep[:, RR_ALOG:RR_ALOG + 4] = inp["ssd_a_log"][:L]
    rowrep[:, RR_BI:RR_BI + 4] = inp["ml_b_i"][:L]
    rowrep[:, RR_BF:RR_BF + 4] = inp["ml_b_f"][:L]
    rowrep = np.ascontiguousarray(np.broadcast_to(rowrep[:, None, :], (L, 128, NRR)))
    colpack = np.zeros((L, 128, NCP), np.float32)
    cw = np.concatenate([inp["ssd_conv_w"][:L], inp["ml_conv_w"][:L]], axis=2)
    cb = np.concatenate([inp["ssd_conv_b"][:L], inp["ml_conv_b"][:L]], axis=1)
    colpack[:, :, CP_CW:CP_CW + 40] = cw.reshape(L, 4, 10, 128).transpose(0, 3, 2, 1).reshape(L, 128, 40)
    colpack[:, :, CP_CB:CP_CB + 10] = cb.reshape(L, 10, 128).transpose(0, 2, 1)
    walaug = np.zeros((L, 33, 128), np.float32)
    walaug[:, 0:16, :] = inp["gla_w_alpha"][:L]
    walaug[:, 32, :] = inp["gla_b_alpha"][:L]
    fing = np.ascontiguousarray(np.broadcast_to(inp["final_norm_g"][None, :], (128, D)))
    return dict(wA=wA, rowrep=rowrep, colpack=colpack, walaug=walaug, fing=fing)


_CACHE = {}


def run(inputs, S, L, ncores):
    key = (S, L)
    if key not in _CACHE:
        nc = bass.Bass("TRN2", target_bir_lowering=False)
        Bld(nc, S, L).build()
        _CACHE[key] = nc
    nc = _CACHE[key]
    inp = {k: np.asarray(v) for k, v in inputs.items()}
    shared = dict(_consts(S))
    shared.update(_prep_weights(inp, L))
    ND = (L + 1) // 2
    NM = max(L // 2, 1)
    shared["w_in"] = inp["w_in"][:L]
    shared["w_branch"] = inp["w_branch"][:L]
    shared["w_out"] = inp["w_out"][:L]
    shared["ffn_w1"] = inp["ffn_w1"][:ND]
    shared["ffn_w3"] = inp["ffn_w3"][:ND]
    shared["ffn_w2"] = inp["ffn_w2"][:ND]
    shared["router_w"] = inp["router_w"][:NM]
    shared["moe_w1"] = inp["moe_w1"][:NM]
    shared["moe_w3"] = inp["moe_w3"][:NM]
    shared["moe_w2"] = inp["moe_w2"][:NM]
    shared = {k: np.ascontiguousarray(v) for k, v in shared.items()}
    maps = []
    for c in range(ncores):
        m = dict(shared)
        m["x"] = np.ascontiguousarray(inp["x"][c, :S])
        maps.append(m)
    res = run_bass_kernel_spmd(nc, maps, core_ids=list(range(ncores)))
    return np.stack([res.results[c]["out"] for c in range(ncores)], axis=0)


def kernel(**inputs):
    return run(inputs, 4096, 4, 8).astype(np.float32)
```

```python
import math
from contextlib import ExitStack

import numpy as np
import ml_dtypes

import concourse.bass as bass
import concourse.mybir as mybir
from concourse.bass_utils import run_bass_kernel_spmd

F32 = mybir.dt.float32
BF16 = mybir.dt.bfloat16
AF = mybir.ActivationFunctionType
ALU = mybir.AluOpType
AX = mybir.AxisListType

D = 1024
NCOL = 7964
DFF = 2816
NE = 8
EPS = 1e-6
NDS = 64

FMW = 18 * 128 + 16
TMO = FMW
NA = FMW + 2048 + 12
RR_MIXG = 0
RR_RETG = 1024
RR_SSDG = 1280
RR_GLAG = 1536
RR_MLG = 1792
RR_DVEC = 2048
RR_DTB = 2304
RR_ALOG = 2308
RR_BI = 2312
RR_BF = 2316
NRA = 2320
RR_FFNG = 2320
NRR = 3344
CP_CW = 0
CP_CB = 40
NCP = 50


class Tk:
    __slots__ = ("w", "r")

    def __init__(self):
        self.w = None
        self.r = {}


class Buf:
    def __init__(self, t, excl=False):
        self.t = t
        self.k = Tk()
        self.excl = excl

    def __getitem__(self, idx):
        return self.t[idx]


class Prog:
    ENG = ("pe", "act", "dve", "pool", "sp")

    def __init__(self, nc, stack):
        self.nc = nc
        self.ops = {e: [] for e in self.ENG}
        self.sems = []
        self.esem = {}
        for e in ("pe", "act", "dve", "pool"):
            self.esem[e] = len(self.sems)
            self.sems.append(stack.enter_context(nc.semaphore("s_" + e)))
        self.cur = [0] * 4
        self.dsem = []
        for i in range(NDS):
            self.dsem.append(len(self.sems))
            self.sems.append(stack.enter_context(nc.semaphore("d%d" % i)))
            self.cur.append(0)
        self.dnext = {"sp": 0, "pool": 0}
        self.drange = {"sp": (0, 24), "pool": (24, NDS)}
        self.seen = {e: {} for e in self.ENG}

    def _waits(self, eng, reads, writes, extra=()):
        need = {}
        for t in reads:
            if t.w is not None:
                k, v = t.w
                if need.get(k, 0) < v:
                    need[k] = v
        for t in writes:
            if t.w is not None:
                k, v = t.w
                if need.get(k, 0) < v:
                    need[k] = v
            for k, v in t.r.items():
                if need.get(k, 0) < v:
                    need[k] = v
        for k, v in extra:
            if need.get(k, 0) < v:
                need[k] = v
        seen = self.seen[eng]
        out = []
        pe_own = self.esem["pe"]
        for k, v in need.items():
            if eng == "pe" and k == pe_own:
                continue
            if seen.get(k, 0) >= v:
                continue
            seen[k] = v
            out.append((k, v))
        return out

    def _track(self, ev, reads, writes):
        k, v = ev
        for t in reads:
            if t.r.get(k, 0) < v:
                t.r[k] = v
        for t in writes:
            t.w = ev
            t.r = {}

    region_on = False
    region_count = 0
    region_limit = 10 ** 9

    def op(self, eng, fn, reads=(), writes=()):
        if self.region_on:
            self.region_count += 1
            if self.region_count > self.region_limit:
                return
        writes = list(writes) + [b for b in reads if b.excl and b not in writes]
        reads = [b.k for b in reads]
        writes = [b.k for b in writes]
        waits = self._waits(eng, reads, writes)
        k = self.esem[eng]
        self.cur[k] += 1
        ev = (k, self.cur[k])
        self.ops[eng].append((waits, fn, k, 1))
        self._track(ev, reads, writes)

    def dma(self, q, out, in_, reads=(), writes=()):
        reads = [b.k for b in reads]
        writes = [b.k for b in writes]
        lo, hi = self.drange[q]
        j = lo + self.dnext[q]
        self.dnext[q] = (self.dnext[q] + 1) % (hi - lo)
        k = self.dsem[j]
        extra = [(k, self.cur[k])] if self.cur[k] > 0 else []
        waits = self._waits(q, reads, writes, extra)
        self.cur[k] += 16
        ev = (k, self.cur[k])
        self.ops[q].append((waits, (lambda e: e.dma_start(out=out, in_=in_)), k, 16))
        self._track(ev, reads, writes)

    def barrier(self, engs=None):
        for eng in (engs or self.ENG):
            seen = self.seen[eng]
            waits = []
            for k, v in enumerate(self.cur):
                if v > 0 and seen.get(k, 0) < v and not (eng == "pe" and k == self.esem["pe"]):
                    seen[k] = v
                    waits.append((k, v))
            self.ops[eng].append((waits, None, None, 0))

    def emit(self):
        nc = self.nc
        sems = self.sems

        def run(name, e):
            for waits, fn, sk, inc in self.ops[name]:
                for k, v in waits:
                    e.wait_ge(sems[k], v)
                if fn is None:
                    continue
                fn(e).then_inc(sems[sk], inc)

        with nc.Block() as block:
            @block.tensor
            def _(e):
                run("pe", e)

            @block.scalar
            def _(e):
                run("act", e)

            @block.vector
            def _(e):
                run("dve", e)

            @block.gpsimd
            def _(e):
                run("pool", e)

            @block.sync
            def _(e):
                run("sp", e)


def bc(ap, shape, axis):
    return ap.unsqueeze(axis).broadcast_to(list(shape))


class Bld:
    def __init__(self, nc, S, depth):
        self.nc = nc
        self.S = S
        self.depth = depth
        self.NT = S // 512
        import os
        self.en = set(os.environ.get('KSEC', 'ret,ssd,gla,ml').split(','))
        self.ph = os.environ.get('KPH', 'ABCD')

    def mm(self, out, lhsT, rhs, start, stop, rd, wr):
        self.P.op("pe", lambda e: e.matmul(out, lhsT=lhsT, rhs=rhs, start=start, stop=stop), rd, wr)

    def tr(self, out, in_, rd, wr):
        ident = self.ident
        self.P.op("pe", lambda e: e.transpose(out=out, in_=in_, identity=ident[:]), list(rd) + [ident], wr)

    def act(self, out, in_, func, rd, wr, scale=1.0, bias=None, accum=None):
        kw = {}
        if bias is not None:
            kw["bias"] = bias
        if accum is not None:
            kw["accum_out"] = accum
        self.P.op("act", lambda e: e.activation(out=out, in_=in_, func=func, scale=scale, **kw), rd, wr)

    def tt(self, eng, out, a, b, op, rd, wr):
        self.P.op(eng, lambda e: e.tensor_tensor(out=out, in0=a, in1=b, op=op), rd, wr)

    def ts(self, eng, out, a, s1, s2, op0, op1, rd, wr):
        if s2 is None:
            self.P.op(eng, lambda e: e.tensor_scalar(out=out, in0=a, scalar1=s1, scalar2=None, op0=op0), rd, wr)
        else:
            self.P.op(eng, lambda e: e.tensor_scalar(out=out, in0=a, scalar1=s1, scalar2=s2, op0=op0, op1=op1), rd, wr)

    def stt(self, eng, out, a, sc, b, op0, op1, rd, wr):
        self.P.op(eng, lambda e: e.scalar_tensor_tensor(out=out, in0=a, scalar=sc, in1=b, op0=op0, op1=op1), rd, wr)

    def cp(self, eng, out, in_, rd, wr):
        if eng == "act":
            self.P.op("act", lambda e: e.copy(out=out, in_=in_), rd, wr)
        else:
            self.P.op(eng, lambda e: e.tensor_copy(out=out, in_=in_), rd, wr)

    def ms(self, eng, ap, val, wr):
        self.P.op(eng, lambda e: e.memset(ap, val), [], wr)

    def psf(self):
        return self.pf_free.pop(0)

    def psb(self):
        return self.pb_free.pop(0)

    def prel(self, *bufs):
        for b in bufs:
            if b in self.pf:
                assert b not in self.pf_free
                self.pf_free.append(b)
            else:
                assert b not in self.pb_free
                self.pb_free.append(b)

    def wf(self, kind, n=1):
        pool = self.pf_free if kind == 'f' else self.pb_free
        while len(pool) < n:
            yield

    def sb(self, st, name, shape, dt):
        self.nid = getattr(self, "nid", 0) + 1
        return Buf(st.enter_context(self.nc.sbuf_tensor("%s_%d" % (name, self.nid), list(shape), dt)))

    def build(self):
        nc = self.nc
        S, L = self.S, self.depth
        ND = (L + 1) // 2
        NM = L // 2
        dr = lambda name, shape, dt=F32, kind="ExternalInput": nc.dram_tensor(name, list(shape), dt, kind=kind).ap()
        self.x_in = dr("x", [S, D])
        self.wA = dr("wA", [L, D, NA])
        self.w_in = dr("w_in", [L, D, NCOL])
        self.wbr = dr("w_branch", [L, 4, 256, D])
        self.wout = dr("w_out", [L, D, D])
        self.rowrep = dr("rowrep", [L, 128, NRR])
        self.colpack = dr("colpack", [L, 128, NCP])
        self.walaug = dr("walaug", [L, 33, 128])
        self.fw1 = dr("ffn_w1", [ND, D, DFF])
        self.fw3 = dr("ffn_w3", [ND, D, DFF])
        self.fw2 = dr("ffn_w2", [ND, DFF, D])
        self.rw = dr("router_w", [max(NM, 1), D, NE])
        self.mw1 = dr("moe_w1", [max(NM, 1), NE, D, DFF])
        self.mw3 = dr("moe_w3", [max(NM, 1), NE, D, DFF])
        self.mw2 = dr("moe_w2", [max(NM, 1), NE, DFF, D])
        self.fing = dr("fing", [128, D])
        self.c32 = dr("c32", [128, 5 * 128 + 8 + 256])
        self.identd = dr("identd", [128, 128], BF16)
        self.cosd = dr("cosT", [128, S])
        self.sind = dr("sinT", [128, S])
        self.out = dr("out", [S, D], F32, "ExternalOutput")
        self.xres = dr("xres", [S, D], F32, "Internal")
        self.hT_d = dr("hT_d", [8, 128, S], BF16, "Internal")
        self.yT_d = dr("yT_d", [8, 128, S], BF16, "Internal")
        self.hT2_d = dr("hT2_d", [8, 128, S], BF16, "Internal")

        with ExitStack() as st0:
            self.P = Prog(nc, st0)
            P = self.P
            self.pf = [Buf(st0.enter_context(nc.psum_tensor("pf%d" % i, [128, 512], F32)), True) for i in range(6)]
            self.pb = [Buf(st0.enter_context(nc.psum_tensor("pb%d" % i, [128, 1024], BF16)), True) for i in range(2)]
            self.pf_free = list(self.pf)
            self.pb_free = list(self.pb)
            self.ident = self.sb(st0, "ident", [128, 128], BF16)
            self.cc = self.sb(st0, "cc", [128, 5 * 128 + 8 + 256], F32)
            P.dma("sp", self.ident[:], self.identd, writes=[self.ident])
            P.dma("sp", self.cc[:], self.c32, writes=[self.cc])
            cc = self.cc
            self.tri = cc[:, 0:128]
            self.neg = cc[:, 128:256]
            self.ones = cc[:, 256:384]
            self.bd01 = cc[:, 384:512]
            self.kapR = cc[:, 640:644]
            self.qdec = cc[:, 644:648]
            self.EMr = cc[:, 648:904]
            self.gate = self.sb(st0, "gate", [128, S // 128, NE], F32)
            self.xrow = [Buf(None) for _ in range(S // 128)]

            for l in range(L):
                xsrc = self.x_in if l == 0 else self.xres
                if 'A' in self.ph:
                    self.phaseA(l, xsrc)
                    P.barrier()
                if 'B' in self.ph:
                    self.phaseB(l, xsrc)
                    P.barrier()
                if 'C' in self.ph:
                    self.phaseC(l)
                    P.barrier()
            if 'D' in self.ph:
                self.phaseD()
            P.barrier()
            P.emit()
        return nc

    def phaseA(self, l, xsrc):
        nc, P, S, NT = self.nc, self.P, self.S, self.NT
        tri, neg, ones, bd01 = self.tri, self.neg, self.ones, self.bd01
        cc = self.cc
        with ExitStack() as st:
            sb = lambda name, shape, dt=F32: self.sb(st, name, shape, dt)
            wA = [sb("wA%d" % k, [128, NA], BF16) for k in range(8)]
            for k in range(8):
                P.dma("pool", wA[k][:], self.wA[l, k * 128:(k + 1) * 128, :], writes=[wA[k]])
            rr = sb("rr", [128, NRA])
            P.dma("sp", rr[:], self.rowrep[l, :, 0:NRA], writes=[rr])
            cpk = sb("cpk", [128, NCP])
            P.dma("sp", cpk[:], self.colpack[l], writes=[cpk])
            wal = sb("wal", [33, 128], BF16)
            P.dma("pool", wal[:], self.walaug[l], writes=[wal])
            arep = sb("arep", [128, 4])
            self.act(arep[:], rr[:, RR_ALOG:RR_ALOG + 4], AF.Exp, [rr], [arep])
            self.ts("dve", arep[:], arep[:], -1.0, None, ALU.mult, None, [arep], [arep])

            xb = [sb("xb%d" % i, [128, D]) for i in range(2)]
            hb = sb("hb", [128, D], BF16)
            hTs = [sb("hT%d" % i, [128, 8, 512], BF16) for i in range(2)]
            cosb = sb("cosb", [128, 512])
            sinb = sb("sinb", [128, 512])
            rtmp = [sb("rtmp%d" % i, [128, 512]) for i in range(2)]
            rqTs = [sb("rqT%d" % i, [128, 2, 512], BF16) for i in range(2)]
            rkTs = [sb("rkT%d" % i, [128, 2, 512], BF16) for i in range(2)]
            raw = [sb("raw%d" % i, [128, 515]) for i in range(2)]
            halo = sb("halo", [128, 10, 3])
            cacc = [sb("cacc%d" % i, [128, 512]) for i in range(2)]
            cvTs = [sb("cvT%d" % i, [128, 10, 512], BF16) for i in range(2)]
            gaTs = [sb("gaT%d" % i, [33, 512], BF16) for i in range(2)]
            st4 = sb("st4", [128, 12])
            smN = sb("smN", [128, 8])
            smR = sb("smR", [128, 16])
            smS = sb("smS", [128, 64])
            smG = sb("smG", [128, 16])
            smM = sb("smM", [128, 64])
            vr = sb("vr", [128, 4, 64], BF16)
            sgr = sb("sgr", [128, 256])
            e1 = sb("e1", [128, 128])
            lap = sb("lap", [128, 4, 64])
            eq = sb("eq", [128, 4, 64])
            ek = sb("ek", [128, 4, 64])
            qpad = sb("qpad", [128, 4, 64], BF16)
            kpad = sb("kpad", [128, 4, 64], BF16)
            qkTg = sb("qkTg", [128, 4, 128], BF16)
            vg = sb("vg", [128, 4, 64], BF16)
            vm = sb("vm", [128, 4, 65], BF16)
            sgm = sb("sgm", [128, 256])
            sgz = sb("sgz", [128, 512])
            ktr = sb("ktr", [128, 256], BF16)
            ktm = sb("ktm", [128, 256], BF16)
            xBt = sb("xBt", [128, 4, 128], BF16)
            Rb = sb("Rb", [128, 4, 128])
            Dm = sb("Dm", [128, 4, 128])
            pTs_ = {m: sb("pT" + m, [128, 4, 128], BF16) for m in "RSGM"}
            vs = sb("vs", [128, 4, 64], BF16)
            vs2 = sb("vs2", [128, 4, 64], BF16)
            ob = {m: [sb("o%s%d" % (m, i), [128, 256]) for i in range(2)] for m in "RSGM"}
            for m in "RSGM":
                ob[m].append(ob[m][1])
            gsgs = {m: sb("gsg" + m, [128, 256]) for m in "RGM"}
            ytok = {m: sb("ytok" + m, [128, 256], BF16) for m in "RSGM"}
            yTs = [sb("yT%d" % i, [128, 8, 128], BF16) for i in range(2)]
            Sr = sb("Sr", [128, 2, 128]); Sr16 = sb("Sr16", [128, 2, 128], BF16)
            Sg = sb("Sg", [128, 2, 128]); Sg16 = sb("Sg16", [128, 2, 128], BF16)
            Sm = sb("Sm", [128, 2, 130]); Sm16 = sb("Sm16", [128, 2, 130], BF16)
            Ss = sb("Ss", [128, 4, 64]); Ss16 = sb("Ss16", [128, 4, 64], BF16)
            EMg = sb("EMg", [128, 2, 128])
            EMm = sb("EMm", [128, 2, 130])
            for b_ in (Sr, Sg, Sm, Ss, EMm, lap, halo):
                self.ms("dve", b_[:], 0.0, [b_])
            for b_ in (Sr16, Sg16, Sm16, Ss16, qpad, kpad, gaTs[0], gaTs[1]):
                self.ms("pool", b_[:], 0.0, [b_])
            for g_ in gaTs:
                self.ms("pool", g_[32:33, :], 1.0, [g_])

            def rstd(out, in_, scale, buf):
                self.act(out, in_, AF.Ln, [buf], [buf], scale=scale, bias=EPS)
                self.act(out, out, AF.Exp, [buf], [buf], scale=-0.5)

            def sigm(out, x_ap, xb_, ob_):
                self.act(out, x_ap, AF.Exp, [xb_], [ob_], scale=-1.0)
                self.act(out, out, AF.Ln, [ob_], [ob_], bias=1.0)
                self.act(out, out, AF.Exp, [ob_], [ob_], scale=-1.0)

            v4 = lambda ap: ap.rearrange("p (h v) -> p h v", h=4)

            def pre_gen(t):
                tsl = slice(t * 512, (t + 1) * 512)
                hT, rqT, rkT, cvT, gaT = hTs[t % 2], rqTs[t % 2], rkTs[t % 2], cvTs[t % 2], gaTs[t % 2]
                for c in range(4):
                    row0 = (t * 4 + c) * 128
                    xt = xb[c % 2]
                    P.dma("sp", xt[:], xsrc[row0:row0 + 128, :], writes=[xt])
                    self.act(hb[:], xt[:], AF.Square, [xt], [hb, smN], accum=smN[:, 0:1])
                    rstd(smN[:, 1:2], smN[:, 0:1], 1.0 / D, smN)
                    self.stt("dve", hb[:], xt[:], smN[:, 1:2], rr[:, RR_MIXG:RR_MIXG + D], ALU.mult, ALU.mult, [xt, smN, rr], [hb])
                    yield from self.wf('b')
                    pt = self.psb()
                    for k in range(8):
                        self.tr(pt[:, k * 128:(k + 1) * 128], hb[:, k * 128:(k + 1) * 128], [hb], [pt])
                    self.cp("act", hT[:, :, c * 128:(c + 1) * 128], pt[:].rearrange("p (k n) -> p k n", k=8), [pt], [hT])
                    self.prel(pt)
                    yield
                P.dma("pool", self.hT_d[:, :, tsl].rearrange("k p n -> p k n"), hT[:], reads=[hT])
                P.dma("sp", cosb[:], self.cosd[:, tsl], writes=[cosb])
                P.dma("sp", sinb[:], self.sind[:, tsl], writes=[sinb])

                def fm(b, M=128):
                    ps = self.psf()
                    for k in range(8):
                        self.mm(ps[0:M, :], wA[k][:, b * 128:b * 128 + M], hT[:, k, :], k == 0, k == 7, [wA[k], hT], [ps])
                    return ps
                for qk, dst in ((0, rqT), (1, rkT)):
                    for p in range(2):
                        yield from self.wf('f')
                        ps1 = fm(qk * 4 + p)
                        self.tt("dve", rtmp[0][:], ps1[:], cosb[:], ALU.mult, [ps1, cosb], [rtmp[0]])
                        self.prel(ps1)
                        yield
                        yield from self.wf('f')
                        ps2 = fm(qk * 4 + 2 + p)
                        self.tt("dve", rtmp[1][:], ps2[:], sinb[:], ALU.mult, [ps2, sinb], [rtmp[1]])
                        self.prel(ps2)
                        self.tt("pool", dst[:, p, :], rtmp[0][:], rtmp[1][:], ALU.add, [rtmp[0], rtmp[1]], [dst])
                        yield
                yield from self.wf('f')
                ps = fm(18, M=16)
                self.cp("act", gaT[0:16, :], ps[0:16, :], [ps], [gaT])
                self.prel(ps)
                for b in range(10):
                    yield from self.wf('f')
                    ps = fm(8 + b)
                    rw_ = raw[b % 2]
                    ac = cacc[b % 2]
                    self.cp("act", rw_[:, 3:515], ps[:], [ps], [rw_])
                    self.prel(ps)
                    self.cp("dve", rw_[:, 0:3], halo[:, b, :], [halo], [rw_])
                    cw = lambda k_: cpk[:, CP_CW + b * 4 + k_:CP_CW + b * 4 + k_ + 1]
                    yield
                    self.ts("dve", ac[:], rw_[:, 0:512], cw(0), cpk[:, CP_CB + b:CP_CB + b + 1], ALU.mult, ALU.add, [rw_, cpk], [ac])
                    for k_ in range(1, 4):
                        yield
                        self.stt("dve", ac[:], rw_[:, k_:k_ + 512], cw(k_), ac[:], ALU.mult, ALU.add, [rw_, cpk, ac], [ac])
                    self.cp("dve", halo[:, b, :], rw_[:, 512:515], [rw_], [halo])
                    sg_ = rtmp[b % 2]
                    sigm(sg_[:], ac[:], ac, sg_)
                    yield
                    self.tt("pool", cvT[:, b, :], ac[:], sg_[:], ALU.mult, [ac, sg_], [cvT])
                    yield


            def run_fg(fg, bg, wts=None):
                fg = list(fg)
                wts = dict(wts or {})
                while fg:
                    for g_ in list(fg):
                        try:
                            for _ in range(wts.get(id(g_), 1)):
                                next(g_)
                        except StopIteration:
                            fg.remove(g_)
                    for g_ in list(bg):
                        try:
                            next(g_)
                        except StopIteration:
                            bg.remove(g_)

            run_fg([pre_gen(0)], [])
            for t in range(NT):
                tsl = slice(t * 512, (t + 1) * 512)
                hT, rqT, rkT, cvT, gaT = hTs[t % 2], rqTs[t % 2], rkTs[t % 2], cvTs[t % 2], gaTs[t % 2]
                bg = [pre_gen(t + 1)] if t + 1 < NT else []
                for c in range(4):
                    csl = slice(c * 128, (c + 1) * 128)

                    def tmproj(j, ncol):
                        ps = self.psf()
                        off = TMO + j * 512
                        for k in range(8):
                            self.mm(ps[:, 0:ncol], hT[:, k, csl], wA[k][:, off:off + ncol], k == 0, k == 7, [hT, wA[k]], [ps])
                        return ps

                    def sec_common():
                        yield from self.wf('f')
                        ps = tmproj(4, 12)
                        self.cp("act", st4[:], ps[:, 0:12], [ps], [st4])
                        self.prel(ps)
                        yield from self.wf('f')
                        ps3 = tmproj(3, 512)
                        sigm(sgz[:], ps3[:], ps3, sgz)
                        self.tt("dve", sgz[:], sgz[:], ps3[:], ALU.mult, [sgz, ps3], [sgz])
                        self.prel(ps3)

                    def sec_ret():
                        sm = smR
                        o1, o2, o3 = ob["R"]
                        gsg = gsgs["R"]
                        yield from self.wf('f')
                        ps = tmproj(0, 512)
                        self.tt("dve", vr[:], v4(ps[:, 0:256]), bc(self.kapR, [128, 4, 64], 2), ALU.mult, [ps, cc], [vr])
                        yield
                        sigm(sgr[:], ps[:, 256:512], ps, sgr)
                        yield
                        self.tt("dve", sgr[:], sgr[:], ps[:, 256:512], ALU.mult, [sgr, ps], [sgr])
                        self.prel(ps)
                        self.tt("pool", gsg[:], sgr[:], rr[:, RR_RETG:RR_RETG + 256], ALU.mult, [sgr, rr], [gsg])
                        yield
                        yield from self.wf('b')
                        ptb = self.psb()
                        for p in range(2):
                            self.tr(ptb[:, p * 128:(p + 1) * 128], rkT[:, p, csl], [rkT], [ptb])
                        self.cp("act", ktr[:], ptb[:, 0:256], [ptb], [ktr])
                        self.prel(ptb)
                        yield
                        yield from self.lin_core(rqT, rkT, csl, ktr, vr, 64, Sr, Sr16, self.EMr.rearrange("p (a b) -> p a b", a=2), [cc], pTs_["R"])
                        pso = self.last_pso["R"] if False else self._pso
                        self.tt("dve", v4(o1[:]), v4(pso[:, 0:256]), bc(self.qdec, [128, 4, 64], 2), ALU.mult, [pso, cc], [o1])
                        self.prel(pso)
                        yield
                        yield from self.head_norm(o1, o2, o3, sm, gsg, ytok["R"])

                    def sec_ssd():
                        sm = smS
                        o1, o2, o3 = ob["S"]
                        self.tt("dve", sm[:, 8:12], st4[:, 0:4], rr[:, RR_DTB:RR_DTB + 4], ALU.add, [st4, rr], [sm])
                        self.act(sm[:, 12:16], sm[:, 8:12], AF.Exp, [sm], [sm])
                        yield
                        self.act(sm[:, 16:20], sm[:, 12:16], AF.Ln, [sm], [sm], bias=1.0)
                        yield
                        self.tt("dve", sm[:, 20:24], sm[:, 16:20], arep[:], ALU.mult, [sm, arep], [sm])
                        yield
                        yield from self.wf('f')
                        psA = self.psf()
                        self.mm(psA[:, 0:4], tri, sm[:, 20:24], True, True, [cc, sm], [psA])
                        self.mm(psA[:, 4:8], ones, sm[:, 20:24], True, True, [cc, sm], [psA])
                        self.tt("dve", Rb[:], bc(tri, [128, 4, 128], 1), bc(sm[:, 20:24], [128, 4, 128], 2), ALU.mult, [cc, sm], [Rb])
                        yield
                        self.cp("act", sm[:, 24:32], psA[:, 0:8], [psA], [sm])
                        self.prel(psA)
                        yield from self.wf('f')
                        psR = self.psf()
                        self.mm(psR[:], ones, Rb[:].rearrange("p h i -> p (h i)"), True, True, [cc, Rb], [psR])
                        yield
                        self.tt("dve", Dm[:], psR[:].rearrange("p (h i) -> p h i", h=4), bc(sm[:, 24:28], [128, 4, 128], 2), ALU.subtract, [psR, sm], [Dm])
                        self.prel(psR)
                        yield
                        self.tt("pool", Dm[:], Dm[:], bc(neg, [128, 4, 128], 1), ALU.add, [Dm, cc], [Dm])
                        yield
                        self.act(Dm[:], Dm[:], AF.Exp, [Dm], [Dm])
                        yield from self.wf('b')
                        ptb = self.psb()
                        for j in range(4):
                            self.tr(ptb[:, j * 128:(j + 1) * 128], cvT[:, j, csl], [cvT], [ptb])
                        self.cp("act", xBt[:], ptb[:, 0:512].rearrange("p (a n) -> p a n", a=4), [ptb], [xBt])
                        self.prel(ptb)
                        yield
                        xtok = xBt[:, 0:2, :].rearrange("p a (r v) -> p (a r) v", r=2)
                        self.tt("dve", vs[:], xtok, bc(sm[:, 16:20], [128, 4, 64], 2), ALU.mult, [xBt, sm], [vs])
                        self.tt("dve", sm[:, 32:36], sm[:, 28:32], sm[:, 24:28], ALU.subtract, [sm], [sm])
                        yield
                        self.act(sm[:, 36:40], sm[:, 32:36], AF.Exp, [sm], [sm])
                        self.act(sm[:, 40:48], sm[:, 24:32], AF.Exp, [sm], [sm])
                        yield from self.wf('f')
                        pss = self.psf()
                        for g in range(2):
                            self.mm(pss[:, g * 128:(g + 1) * 128], cvT[:, 2 + g, csl], cvT[:, 4 + g, csl], True, True, [cvT], [pss])
                        yield
                        self.tt("dve", vs2[:], vs[:], bc(sm[:, 36:40], [128, 4, 64], 2), ALU.mult, [vs, sm], [vs2])
                        pTs = pTs_["S"]
                        for g in range(2):
                            self.tt("dve", pTs[:, 2 * g:2 * g + 2, :], bc(pss[:, g * 128:(g + 1) * 128], [128, 2, 128], 1), Dm[:, 2 * g:2 * g + 2, :], ALU.mult, [pss, Dm], [pTs])
                        self.prel(pss)
                        yield
                        yield from self.wf('f', 2)
                        psy = self.psf()
                        for h in range(4):
                            self.mm(psy[:, h * 64:(h + 1) * 64], pTs[:, h, :], vs[:, h, :], True, True, [pTs, vs], [psy])
                        for g in range(2):
                            self.mm(psy[:, 256 + g * 128:256 + (g + 1) * 128], cvT[:, 4 + g, csl], Ss16[:, 2 * g:2 * g + 2, :].rearrange("p a v -> p (a v)"), True, True, [cvT, Ss16], [psy])
                        psu = self.psf()
                        for g in range(2):
                            self.mm(psu[:, g * 128:(g + 1) * 128], xBt[:, 2 + g, :], vs2[:, 2 * g:2 * g + 2, :].rearrange("p a v -> p (a v)"), True, True, [xBt, vs2], [psu])
                        yield
                        self.tt("dve", Ss[:], Ss[:], bc(sm[:, 44:48], [128, 4, 64], 2), ALU.mult, [Ss, sm], [Ss])
                        yield
                        self.tt("dve", Ss[:], Ss[:], v4(psu[:, 0:256]), ALU.add, [Ss, psu], [Ss])
                        self.prel(psu)
                        yield
                        self.cp("act", Ss16[:], Ss[:], [Ss], [Ss16])
                        self.tt("dve", v4(o1[:]), v4(psy[:, 256:512]), bc(sm[:, 40:44], [128, 4, 64], 2), ALU.mult, [psy, sm], [o1])
                        yield
                        self.tt("dve", o1[:], o1[:], psy[:, 0:256], ALU.add, [o1, psy], [o1])
                        self.prel(psy)
                        self.tt("pool", v4(o2[:]), xtok, v4(rr[:, RR_DVEC:RR_DVEC + 256]), ALU.mult, [xBt, rr], [o2])
                        yield
                        self.tt("dve", o1[:], o1[:], o2[:], ALU.add, [o1, o2], [o1])
                        yield
                        self.tt("dve", o1[:], o1[:], sgz[:, 0:256], ALU.mult, [o1, sgz], [o1])
                        yield
                        self.act(o3[:], o1[:], AF.Square, [o1], [o3, sm], accum=sm[:, 48:49])
                        yield
                        rstd(sm[:, 49:50], sm[:, 48:49], 1.0 / 256, sm)
                        yield
                        self.stt("dve", ytok["S"][:], o1[:], sm[:, 49:50], rr[:, RR_SSDG:RR_SSDG + 256], ALU.mult, ALU.mult, [o1, sm, rr], [ytok["S"]])

                    def sec_gla():
                        sm = smG
                        o1, o2, o3 = ob["G"]
                        gsg = gsgs["G"]
                        yield from self.wf('f')
                        psa = self.psf()
                        self.mm(psa[:, 0:128], gaT[0:33, csl], wal[:], True, True, [gaT, wal], [psa])
                        self.act(e1[:], psa[:, 0:128], AF.Exp, [psa], [e1], scale=-1.0)
                        self.prel(psa)
                        yield
                        self.act(e1[:], e1[:], AF.Ln, [e1], [e1], bias=1.0)
                        yield
                        self.ts("dve", lap[:, :, 0:32], e1[:].rearrange("p (h d) -> p h d", h=4), -1.0 / 16.0, None, ALU.mult, None, [e1], [lap])
                        self.tt("pool", gsg[:], sgz[:, 256:512], rr[:, RR_GLAG:RR_GLAG + 256], ALU.mult, [sgz, rr], [gsg])
                        yield
                        yield from self.wf('f')
                        psg = self.psf()
                        self.mm(psg[:, 0:256], tri, lap[:].rearrange("p h d -> p (h d)"), True, True, [cc, lap], [psg])
                        for p in range(2):
                            self.mm(psg[:, 256 + p:257 + p], lap[:, 2 * p:2 * p + 2, :].rearrange("p a d -> p (a d)"), ones[:, 0:1], True, True, [lap, cc], [psg])
                        yield
                        self.act(eq[:], v4(psg[:, 0:256]), AF.Exp, [psg], [eq])
                        self.act(ek[:], v4(psg[:, 0:256]), AF.Exp, [psg], [ek], scale=-1.0)
                        self.act(sm[:, 0:2], psg[:, 256:258], AF.Exp, [psg], [sm])
                        self.prel(psg)
                        yield
                        for p in range(2):
                            self.ts("dve", EMg[:, p, :], bd01, sm[:, p:p + 1], None, ALU.mult, None, [cc, sm], [EMg])
                        yield from self.wf('f')
                        ps1 = tmproj(1, 512)
                        yield
                        self.stt("dve", qpad[:, :, 0:32], ps1[:, 0:128].rearrange("p (h d) -> p h d", h=4), 32.0 ** -0.5, eq[:, :, 0:32], ALU.mult, ALU.mult, [ps1, eq], [qpad])
                        yield
                        self.tt("dve", kpad[:, :, 0:32], ps1[:, 128:256].rearrange("p (h d) -> p h d", h=4), ek[:, :, 0:32], ALU.mult, [ps1, ek], [kpad])
                        yield
                        self.cp("act", vg[:], v4(ps1[:, 256:512]), [ps1], [vg])
                        self.prel(ps1)
                        yield from self.wf('b')
                        ptb = self.psb()
                        for p in range(2):
                            self.tr(ptb[:, p * 128:(p + 1) * 128], qpad[:, 2 * p:2 * p + 2, :].rearrange("p a d -> p (a d)"), [qpad], [ptb])
                            self.tr(ptb[:, 256 + p * 128:256 + (p + 1) * 128], kpad[:, 2 * p:2 * p + 2, :].rearrange("p a d -> p (a d)"), [kpad], [ptb])
                        self.cp("act", qkTg[:], ptb[:, 0:512].rearrange("p (a n) -> p a n", a=4), [ptb], [qkTg])
                        self.prel(ptb)
                        yield
                        yield from self.lin_core(qkTg, qkTg, None, kpad[:].rearrange("p h d -> p (h d)"), vg, 64, Sg, Sg16, EMg[:], [EMg], pTs_["G"], koff=2, ktok_buf=kpad)
                        pso = self._pso
                        self.cp("act", o1[:], pso[:, 0:256], [pso], [o1])
                        self.prel(pso)
                        yield
                        yield from self.head_norm(o1, o2, o3, sm, gsg, ytok["G"])

                    def sec_ml():
                        sm = smM
                        o1, o2, o3 = ob["M"]
                        gsg = gsgs["M"]
                        yield from self.wf('f')
                        ps2 = tmproj(2, 512)
                        sigm(sgm[:], ps2[:, 256:512], ps2, sgm)
                        yield
                        self.tt("pool", gsg[:], sgm[:], rr[:, RR_MLG:RR_MLG + 256], ALU.mult, [sgm, rr], [gsg])
                        self.tt("dve", sm[:, 8:12], st4[:, 8:12], rr[:, RR_BF:RR_BF + 4], ALU.add, [st4, rr], [sm])
                        yield
                        self.act(sm[:, 12:16], sm[:, 8:12], AF.Exp, [sm], [sm], scale=-1.0)
                        yield
                        self.act(sm[:, 16:20], sm[:, 12:16], AF.Ln, [sm], [sm], bias=1.0)
                        yield
                        yield from self.wf('f')
                        psb_ = self.psf()
                        self.mm(psb_[:, 0:4], tri, sm[:, 16:20], True, True, [cc, sm], [psb_])
                        self.mm(psb_[:, 4:8], ones, sm[:, 16:20], True, True, [cc, sm], [psb_])
                        self.tt("dve", sm[:, 20:24], st4[:, 4:8], rr[:, RR_BI:RR_BI + 4], ALU.add, [st4, rr], [sm])
                        yield
                        self.tt("dve", sm[:, 20:24], sm[:, 20:24], psb_[:, 0:4], ALU.add, [sm, psb_], [sm])
                        yield
                        self.act(sm[:, 24:28], sm[:, 20:24], AF.Exp, [sm], [sm], bias=math.log(0.125))
                        self.act(sm[:, 28:36], psb_[:, 0:8], AF.Exp, [psb_], [sm], scale=-1.0)
                        self.prel(psb_)
                        yield
                        self.tt("dve", vm[:, :, 0:64], v4(ps2[:, 0:256]), bc(sm[:, 24:28], [128, 4, 64], 2), ALU.mult, [ps2, sm], [vm])
                        self.prel(ps2)
                        yield
                        self.cp("dve", vm[:, :, 64:65], sm[:, 24:28].unsqueeze(2), [sm], [vm])
                        for p in range(2):
                            for r in range(2):
                                hh = 2 * p + r
                                self.ts("dve", EMm[r * 64:(r + 1) * 64, p, r * 65:(r + 1) * 65], ones[r * 64:(r + 1) * 64, 0:65], sm[r * 64:(r + 1) * 64, 32 + hh:33 + hh], None, ALU.mult, None, [cc, sm], [EMm])
                            yield
                        yield from self.wf('b')
                        ptb = self.psb()
                        for p in range(2):
                            self.tr(ptb[:, p * 128:(p + 1) * 128], cvT[:, 8 + p, csl], [cvT], [ptb])
                        self.cp("act", ktm[:], ptb[:, 0:256], [ptb], [ktm])
                        self.prel(ptb)
                        yield
                        yield from self.lin_core(cvT, cvT, csl, ktm, vm, 65, Sm, Sm16, EMm[:], [EMm], pTs_["M"], qoff=6, koff=8)
                        pso = self._pso
                        pso3 = pso[:, 0:260].rearrange("p (h v) -> p h v", h=4)
                        self.tt("dve", sm[:, 36:40], pso3[:, :, 64], sm[:, 28:32], ALU.mult, [pso, sm], [sm])
                        yield
                        self.stt("dve", sm[:, 40:44], sm[:, 36:40], -1.0, sm[:, 36:40], ALU.mult, ALU.max, [sm], [sm])
                        yield
                        self.ts("dve", sm[:, 40:44], sm[:, 40:44], 1.0, None, ALU.max, None, [sm], [sm])
                        yield
                        self.P.op("dve", lambda e: e.reciprocal(out=sm[:, 44:48], in_=sm[:, 40:44]), [sm], [sm])
                        yield
                        self.tt("dve", sm[:, 44:48], sm[:, 44:48], sm[:, 28:32], ALU.mult, [sm], [sm])
                        yield
                        self.tt("dve", v4(o1[:]), pso3[:, :, 0:64], bc(sm[:, 44:48], [128, 4, 64], 2), ALU.mult, [pso, sm], [o1])
                        self.prel(pso)
                        yield
                        yield from self.head_norm(o1, o2, o3, sm, gsg, ytok["M"])

                    gens = []
                    wts = {}
                    if 'ssd' in self.en:
                        gens.append(sec_ssd())
                        wts[id(gens[-1])] = 2
                    if 'ml' in self.en:
                        gens.append(sec_ml())
                        wts[id(gens[-1])] = 2
                    if 'ret' in self.en:
                        gens.append(sec_ret())
                    if 'gla' in self.en:
                        gens.append(sec_gla())
                    def sec_y():
                        yT = yTs[c % 2]
                        yield from self.wf('b')
                        ptb = self.psb()
                        for j, m in enumerate("RSGM"):
                            for vc in range(2):
                                self.tr(ptb[:, (2 * j + vc) * 128:(2 * j + vc + 1) * 128], ytok[m][:, vc * 128:(vc + 1) * 128], [ytok[m]], [ptb])
                        self.cp("act", yT[:], ptb[:].rearrange("p (k n) -> p k n", k=8), [ptb], [yT])
                        self.prel(ptb)
                        P.dma("pool", self.yT_d[:, :, t * 512 + c * 128:t * 512 + (c + 1) * 128].rearrange("k p n -> p k n"), yT[:], reads=[yT])

                    run_fg([sec_common()], bg)
                    run_fg(gens, bg, wts)
                    run_fg([sec_y()], bg)
                run_fg(bg, [])

    def lin_core(self, qT, kT, csl, ktok, v, dv, S_, S16, EM, EMb, pTb, qoff=0, koff=0, ktok_buf=None):
        tri = self.tri
        cc = self.cc
        if csl is None:
            qa = lambda p, r: qT[r * 64:(r + 1) * 64, qoff + p, :]
            ka = lambda p, r: kT[r * 64:(r + 1) * 64, koff + p, :]
        else:
            qa = lambda p, r: qT[r * 64:(r + 1) * 64, qoff + p, csl]
            ka = lambda p, r: kT[r * 64:(r + 1) * 64, koff + p, csl]
        yield from self.wf('f', 2)
        pssr = [self.psf(), self.psf()]
        for h in range(4):
            p, r = h // 2, h % 2
            self.mm(pssr[r][:, p * 128:(p + 1) * 128], ka(p, r), qa(p, r), True, True, [kT, qT], [pssr[r]])
        yield
        for r in range(2):
            self.tt("dve", pTb[:, r:4:2, :], pssr[r][:, 0:256].rearrange("p (a i) -> p a i", a=2), bc(tri, [128, 2, 128], 1), ALU.mult, [pssr[r], cc], [pTb])
            self.prel(pssr[r])
            yield
        yield from self.wf('f', 2)
        pso = self.psf()
        for h in range(4):
            p, r = h // 2, h % 2
            self.mm(pso[:, h * dv:(h + 1) * dv], pTb[:, h, :], v[:, h, :], True, False, [pTb, v], [pso])
            self.mm(pso[:, h * dv:(h + 1) * dv], qa(p, r), S16[r * 64:(r + 1) * 64, p, r * dv:(r + 1) * dv], False, True, [qT, S16], [pso])
        psu = self.psf()
        kb = ktok_buf if ktok_buf is not None else ktok
        for p in range(2):
            self.mm(psu[:, p * 2 * dv:(p + 1) * 2 * dv], ktok[:, p * 128:(p + 1) * 128], v[:, 2 * p:2 * p + 2, :].rearrange("p a v -> p (a v)"), True, True, [kb, v], [psu])
        yield
        self.tt("dve", S_[:], S_[:], psu[:, 0:4 * dv].rearrange("p (a v) -> p a v", a=2), ALU.add, [S_, psu], [S_])
        self.prel(psu)
        yield
        self.tt("dve", S_[:], S_[:], EM, ALU.mult, [S_] + list(EMb), [S_])
        yield
        self.cp("act", S16[:], S_[:], [S_], [S16])
        self._pso = pso

    def head_norm(self, o1, o2, o3, sm, gsg, yout):
        self.act(o2[:], o1[:], AF.Square, [o1], [o2])
        yield
        self.P.op("dve", lambda e: e.tensor_reduce(out=sm[:, 4:8], in_=o2[:].rearrange("p (h v) -> p h v", h=4), axis=AX.X, op=ALU.add), [o2], [sm])
        yield
        self.act(sm[:, 4:8], sm[:, 4:8], AF.Ln, [sm], [sm], scale=1.0 / 64, bias=EPS)
        yield
        self.act(sm[:, 4:8], sm[:, 4:8], AF.Exp, [sm], [sm], scale=-0.5)
        yield
        self.tt("dve", o3[:].rearrange("p (h v) -> p h v", h=4), o1[:].rearrange("p (h v) -> p h v", h=4), bc(sm[:, 4:8], [128, 4, 64], 2), ALU.mult, [o1, sm], [o3])
        yield
        self.tt("dve", yout[:], o3[:], gsg[:], ALU.mult, [o3, gsg], [yout])


    def phaseB(self, l, xsrc):
        nc, P, S, NT = self.nc, self.P, self.S, self.NT
        moe = (l % 2 == 1)
        with ExitStack() as st:
            sb = lambda name, shape, dt=F32: self.sb(st, name, shape, dt)
            wG = [sb("wG%d" % k, [128, 4096], BF16) for k in range(8)]
            for k in range(8):
                P.dma("pool", wG[k][:], self.w_in[l, k * 128:(k + 1) * 128, 3868:7964], writes=[wG[k]])
            wb = [sb("wb%d" % j, [128, D], BF16) for j in range(8)]
            for j in range(8):
                n_, vc = j // 2, j % 2
                P.dma("pool", wb[j][:], self.wbr[l, n_, vc * 128:(vc + 1) * 128, :], writes=[wb[j]])
            wo = [sb("wo%d" % k, [128, D], BF16) for k in range(8)]
            for k in range(8):
                P.dma("pool", wo[k][:], self.wout[l, k * 128:(k + 1) * 128, :], writes=[wo[k]])
            gr = sb("grB", [128, D])
            P.dma("sp", gr[:], self.rowrep[l, :, RR_FFNG:RR_FFNG + D], writes=[gr])
            if moe:
                wr = sb("wr", [128, 8, NE], BF16)
                P.dma("pool", wr[:], self.rw[l // 2].rearrange("(k p) e -> p k e", p=128), writes=[wr])
            hTs = [sb("hTB%d" % i, [128, 8, 512], BF16) for i in range(2)]
            yTs = [sb("yTB%d" % i, [128, 8, 512], BF16) for i in range(2)]
            mg = sb("mg", [128, 8, 512], BF16)
            sg = [sb("sg%d" % i, [128, 512]) for i in range(2)]
            macc = sb("macc", [128, 512])
            xt = [sb("xtB%d" % i, [128, D]) for i in range(2)]
            sqj = sb("sqjB", [128, D], BF16)
            hb = sb("hbB", [128, D], BF16)
            hT2 = sb("hT2B", [128, 8, 512], BF16)
            sm = sb("smB", [128, 64])
            lg = sb("lg", [128, 4, NE])
            for t in range(NT):
                tsl = slice(t * 512, (t + 1) * 512)
                hT = hTs[t % 2]
                yT = yTs[t % 2]
                P.dma("sp", hT[:], self.hT_d[:, :, tsl].rearrange("k p n -> p k n"), writes=[hT])
                P.dma("sp", yT[:], self.yT_d[:, :, tsl].rearrange("k p n -> p k n"), writes=[yT])
                for dc in range(8):
                    for n_ in range(4):
                        psg = self.psf()
                        for k in range(8):
                            self.mm(psg[:], wG[k][:, n_ * D + dc * 128:n_ * D + (dc + 1) * 128], hT[:, k, :], k == 0, k == 7, [wG[k], hT], [psg])
                        psb_ = self.psf()
                        for vc in range(2):
                            self.mm(psb_[:], wb[n_ * 2 + vc][:, dc * 128:(dc + 1) * 128], yT[:, n_ * 2 + vc, :], vc == 0, vc == 1, [wb[n_ * 2 + vc], yT], [psb_])
                        s_ = sg[n_ % 2]
                        self.act(s_[:], psg[:], AF.Sigmoid, [psg], [s_])
                        self.prel(psg)
                        if n_ == 0:
                            self.tt("dve", macc[:], s_[:], psb_[:], ALU.mult, [s_, psb_], [macc])
                            self.prel(psb_)
                        else:
                            self.tt("dve", s_[:], s_[:], psb_[:], ALU.mult, [s_, psb_], [s_])
                            self.prel(psb_)
                            if n_ < 3:
                                self.tt("pool", macc[:], macc[:], s_[:], ALU.add, [macc, s_], [macc])
                            else:
                                self.tt("dve", mg[:, dc, :], macc[:], s_[:], ALU.add, [macc, s_], [mg])
                for c in range(4):
                    row0 = (t * 4 + c) * 128
                    csl = slice(c * 128, (c + 1) * 128)
                    x_ = xt[c % 2]
                    P.dma("sp", x_[:], xsrc[row0:row0 + 128, :], writes=[x_])
                    for hf in range(2):
                        ps = self.psf()
                        for k in range(8):
                            self.mm(ps[:], mg[:, k, csl], wo[k][:, hf * 512:(hf + 1) * 512], k == 0, k == 7, [mg, wo[k]], [ps])
                        self.tt("dve", x_[:, hf * 512:(hf + 1) * 512], x_[:, hf * 512:(hf + 1) * 512], ps[:], ALU.add, [x_, ps], [x_])
                        self.prel(ps)
                    P.dma("pool", self.xres[row0:row0 + 128, :], x_[:], reads=[x_], writes=[self.xrow[t * 4 + c]])
                    self.act(sqj[:], x_[:], AF.Square, [x_], [sqj, sm], accum=sm[:, 0:1])
                    self.act(sm[:, 1:2], sm[:, 0:1], AF.Sqrt, [sm], [sm], scale=1.0 / D, bias=EPS)
                    self.P.op("dve", lambda e: e.reciprocal(out=sm[:, 2:3], in_=sm[:, 1:2]), [sm], [sm])
                    self.stt("dve", hb[:], x_[:], sm[:, 2:3], gr[:], ALU.mult, ALU.mult, [x_, sm, gr], [hb])
                    pt = self.psb()
                    for k in range(8):
                        self.tr(pt[:, k * 128:(k + 1) * 128], hb[:, k * 128:(k + 1) * 128], [hb], [pt])
                    self.cp("act", hT2[:, :, csl], pt[:].rearrange("p (k n) -> p k n", k=8), [pt], [hT2])
                    self.prel(pt)
                    if moe:
                        ps = self.psf()
                        for k in range(8):
                            self.mm(ps[:, 0:NE], hT2[:, k, csl], wr[:, k, :], k == 0, k == 7, [hT2, wr], [ps])
                        self.cp("act", lg[:, c, :], ps[:, 0:NE], [ps], [lg])
                        self.prel(ps)
                P.dma("pool", self.hT2_d[:, :, tsl].rearrange("k p n -> p k n"), hT2[:], reads=[hT2])
                if moe:
                    self.top2(lg, sm, self.gate[:, t * 4:(t + 1) * 4, :], st, t)

    def top2(self, lg, sm, gout, st, t):
        if t == 0:
            self.t2 = [self.sb(st, "t2_%d" % i, [128, 4, NE], F32) for i in range(4)]
            self.t2s = self.sb(st, "t2s", [128, 16], F32)
        m1b, l2, m2b, tmp = self.t2
        s_ = self.t2s
        gate = self.gate
        red = lambda out, in_: self.P.op("dve", lambda e: e.tensor_reduce(out=out, in_=in_, axis=AX.X, op=ALU.max), [lg, l2], [s_])
        red(s_[:, 0:4], lg[:])
        self.tt("dve", m1b[:], lg[:], bc(s_[:, 0:4], [128, 4, NE], 2), ALU.is_equal, [lg, s_], [m1b])
        self.stt("dve", l2[:], m1b[:], -1e30, lg[:], ALU.mult, ALU.add, [m1b, lg], [l2])
        red(s_[:, 4:8], l2[:])
        self.tt("dve", m2b[:], l2[:], bc(s_[:, 4:8], [128, 4, NE], 2), ALU.is_equal, [l2, s_], [m2b])
        self.tt("dve", s_[:, 8:12], s_[:, 4:8], s_[:, 0:4], ALU.subtract, [s_], [s_])
        self.act(s_[:, 8:12], s_[:, 8:12], AF.Exp, [s_], [s_])
        self.ts("dve", s_[:, 8:12], s_[:, 8:12], 1.0, None, ALU.add, None, [s_], [s_])
        self.P.op("dve", lambda e: e.reciprocal(out=s_[:, 12:16], in_=s_[:, 8:12]), [s_], [s_])
        self.ts("dve", s_[:, 8:12], s_[:, 12:16], -1.0, 1.0, ALU.mult, ALU.add, [s_], [s_])
        self.tt("dve", m1b[:], m1b[:], bc(s_[:, 12:16], [128, 4, NE], 2), ALU.mult, [m1b, s_], [m1b])
        self.tt("dve", m2b[:], m2b[:], bc(s_[:, 8:12], [128, 4, NE], 2), ALU.mult, [m2b, s_], [m2b])
        self.tt("dve", gout, m1b[:], m2b[:], ALU.add, [m1b, m2b], [gate])

    def phaseC(self, l):
        nc, P, S, NT = self.nc, self.P, self.S, self.NT
        moe = (l % 2 == 1)
        li = l // 2
        if moe:
            items = [(e, hf) for e in range(NE) for hf in range(2)]
        else:
            items = [(None, 0), (None, 1)]
        HF = DFF // 2
        with ExitStack() as st:
            sb = lambda name, shape, dt=F32: self.sb(st, name, shape, dt)
            W1 = [[sb("W1_%d_%d" % (i, k), [128, HF], BF16) for k in range(8)] for i in range(2)]
            W3 = [[sb("W3_%d_%d" % (i, k), [128, HF], BF16) for k in range(8)] for i in range(2)]
            W2 = [[sb("W2_%d_%d" % (i, f), [128, D], BF16) for f in range(11)] for i in range(2)]
            hT2 = [sb("hT2C%d" % i, [128, 8, 512], BF16) for i in range(2)]
            h1T = sb("h1T", [128, 11, 512], BF16)
            gs = [sb("gs%d" % i, [128, 512]) for i in range(2)]
            xt = [sb("xtC%d" % i, [128, D]) for i in range(6)]
            xi = 0
            hi = 0

            def load_w(idx, slot):
                e, hf = items[idx]
                if e is None:
                    s1, s3, s2 = self.fw1[li], self.fw3[li], self.fw2[li]
                else:
                    s1, s3, s2 = self.mw1[li, e], self.mw3[li, e], self.mw2[li, e]
                for k in range(8):
                    P.dma("pool", W1[slot][k][:], s1[k * 128:(k + 1) * 128, hf * HF:(hf + 1) * HF], writes=[W1[slot][k]])
                    P.dma("pool", W3[slot][k][:], s3[k * 128:(k + 1) * 128, hf * HF:(hf + 1) * HF], writes=[W3[slot][k]])
                for f in range(11):
                    P.dma("pool", W2[slot][f][:], s2[hf * HF + f * 128:hf * HF + (f + 1) * 128, :], writes=[W2[slot][f]])

            load_w(0, 0)
            for idx, (e, hf) in enumerate(items):
                slot = idx % 2
                if idx + 1 < len(items):
                    load_w(idx + 1, (idx + 1) % 2)
                for t in range(NT):
                    tsl = slice(t * 512, (t + 1) * 512)
                    h_ = hT2[hi % 2]; hi += 1
                    P.dma("sp", h_[:], self.hT2_d[:, :, tsl].rearrange("k p n -> p k n"), writes=[h_])
                    for f in range(11):
                        pa = self.psf()
                        for k in range(8):
                            self.mm(pa[:], W1[slot][k][:, f * 128:(f + 1) * 128], h_[:, k, :], k == 0, k == 7, [W1[slot][k], h_], [pa])
                        pb_ = self.psf()
                        for k in range(8):
                            self.mm(pb_[:], W3[slot][k][:, f * 128:(f + 1) * 128], h_[:, k, :], k == 0, k == 7, [W3[slot][k], h_], [pb_])
                        g_ = gs[f % 2]
                        self.act(g_[:], pa[:], AF.Silu, [pa], [g_])
                        self.tt("dve", h1T[:, f, :], g_[:], pb_[:], ALU.mult, [g_, pb_], [h1T])
                        self.prel(pa, pb_)
                    for c in range(4):
                        row0 = (t * 4 + c) * 128
                        x_ = xt[xi % 6]; xi += 1
                        P.dma("sp", x_[:], self.xres[row0:row0 + 128, :], reads=[self.xrow[t * 4 + c]], writes=[x_])
                        for hh in range(2):
                            ps = self.psf()
                            for f in range(11):
                                self.mm(ps[:], h1T[:, f, c * 128:(c + 1) * 128], W2[slot][f][:, hh * 512:(hh + 1) * 512], f == 0, f == 10, [h1T, W2[slot][f]], [ps])
                            if e is None:
                                self.tt("dve", x_[:, hh * 512:(hh + 1) * 512], x_[:, hh * 512:(hh + 1) * 512], ps[:], ALU.add, [x_, ps], [x_])
                            else:
                                self.stt("dve", x_[:, hh * 512:(hh + 1) * 512], ps[:], self.gate[:, t * 4 + c, e:e + 1], x_[:, hh * 512:(hh + 1) * 512], ALU.mult, ALU.add, [ps, self.gate, x_], [x_])
                            self.prel(ps)
                        P.dma("pool", self.xres[row0:row0 + 128, :], x_[:], reads=[x_], writes=[self.xrow[t * 4 + c]])

    def phaseD(self):
        P, S = self.P, self.S
        with ExitStack() as st:
            sb = lambda name, shape, dt=F32: self.sb(st, name, shape, dt)
            g = sb("gD", [128, D])
            P.dma("sp", g[:], self.fing, writes=[g])
            xt = [sb("xtD%d" % i, [128, D]) for i in range(2)]
            yo = [sb("yoD%d" % i, [128, D]) for i in range(2)]
            sqj = sb("sqjD", [128, D], BF16)
            sm = sb("smD", [128, 8])
            for c in range(S // 128):
                x_ = xt[c % 2]
                y_ = yo[c % 2]
                P.dma("sp", x_[:], self.xres[c * 128:(c + 1) * 128, :], writes=[x_])
                self.act(sqj[:], x_[:], AF.Square, [x_], [sqj, sm], accum=sm[:, 0:1])
                self.act(sm[:, 1:2], sm[:, 0:1], AF.Sqrt, [sm], [sm], scale=1.0 / D, bias=EPS)
                self.P.op("dve", lambda e: e.reciprocal(out=sm[:, 2:3], in_=sm[:, 1:2]), [sm], [sm])
                self.stt("dve", y_[:], x_[:], sm[:, 2:3], g[:], ALU.mult, ALU.mult, [x_, sm, g], [y_])
                P.dma("pool", self.out[c * 128:(c + 1) * 128, :], y_[:], reads=[y_])


def _consts(S):
    j = np.arange(128)
    tri = (j[:, None] <= j[None, :]).astype(np.float32)
    neg = np.where(j[:, None] <= j[None, :], 0.0, -1e30).astype(np.float32)
    ones = np.ones((128, 128), np.float32)
    bd = ((j[:, None] // 64) == (j[None, :] // 64)).astype(np.float32)
    lg = np.log1p(-np.exp2(-5.0 - np.arange(4, dtype=np.float64)))
    kap = (np.exp(-(j[:, None] + 1.0) * lg[None, :]) * 0.125).astype(np.float32)
    qd = np.exp((j[:, None] + 1.0) * lg[None, :]).astype(np.float32)
    em = np.zeros((128, 2, 128), np.float32)
    for p in range(2):
        for r in range(2):
            em[r * 64:(r + 1) * 64, p, r * 64:(r + 1) * 64] = np.exp(128.0 * lg[2 * p + r])
    c32 = np.concatenate([tri, neg, ones, bd, np.zeros((128, 128), np.float32), kap, qd, em.reshape(128, 256)], axis=1)
    half = 32
    inv = (10000.0 ** (-np.arange(half, dtype=np.float32) / half)).astype(np.float32)
    ang = np.arange(S, dtype=np.float32)[None, :] * inv[:, None]
    cos = np.cos(ang).astype(np.float32)
    sin = np.sin(ang).astype(np.float32)
    cosT = np.concatenate([cos, cos, cos, cos], axis=0)
    sinT = np.concatenate([-sin, sin, -sin, sin], axis=0)
    ident = np.eye(128, dtype=np.float32).astype(ml_dtypes.bfloat16)
    return dict(c32=np.ascontiguousarray(c32), cosT=np.ascontiguousarray(cosT), sinT=np.ascontiguousarray(sinT), identd=ident)


def _prep_weights(inp, L):
    w_in = inp["w_in"][:L]
    o = np.cumsum([0, 256, 256, 256, 256, 256, 768, 4, 128, 128, 256, 256, 16, 512, 256, 256, 4, 4, 4096])
    (RQ, RK, RV, RG, SZ, SX, SDT, GQ, GK, GV, GR, GA, MQK, MV, MO, MI, MF, MG) = o[:18]

    def swap(c0):
        idx = []
        for h in range(4):
            idx += list(range(c0 + h * 64 + 32, c0 + h * 64 + 64)) + list(range(c0 + h * 64, c0 + h * 64 + 32))
        return idx
    cols = (list(range(RQ, RQ + 256)) + swap(RQ) + list(range(RK, RK + 256)) + swap(RK)
            + list(range(SX, SX + 768)) + list(range(MQK, MQK + 512)) + list(range(GA, GA + 16))
            + list(range(RV, RV + 256)) + list(range(RG, RG + 256))
            + list(range(GQ, GQ + 128)) + list(range(GK, GK + 128)) + list(range(GV, GV + 256))
            + list(range(MV, MV + 256)) + list(range(MO, MO + 256))
            + list(range(SZ, SZ + 256)) + list(range(GR, GR + 256))
            + list(range(SDT, SDT + 4)) + list(range(MI, MI + 4)) + list(range(MF, MF + 4)))
    assert len(cols) == NA
    wA = np.ascontiguousarray(w_in[:, :, cols])
    rowrep = np.zeros((L, NRR), np.float32)
    rowrep[:, RR_MIXG:RR_MIXG + D] = inp["mix_norm_g"][:L]
    rowrep[:, RR_FFNG:RR_FFNG + D] = inp["ffn_norm_g"][:L]
    rowrep[:, RR_RETG:RR_RETG + 256] = inp["ret_norm_g"][:L]
    rowrep[:, RR_SSDG:RR_SSDG + 256] = inp["ssd_norm_g"][:L]
    rowrep[:, RR_GLAG:RR_GLAG + 256] = inp["gla_norm_g"][:L]
    rowrep[:, RR_MLG:RR_MLG + 256] = inp["ml_norm_g"][:L]
    rowrep[:, RR_DVEC:RR_DVEC + 256] = np.repeat(inp["ssd_d"][:L], 64, axis=1)
    rowrep[:, RR_DTB:RR_DTB + 4] = inp["ssd_dt_bias"][:L]
    rowrep[:, RR_ALOG:RR_ALOG + 4] = inp["ssd_a_log"][:L]
    rowrep[:, RR_BI:RR_BI + 4] = inp["ml_b_i"][:L]
    rowrep[:, RR_BF:RR_BF + 4] = inp["ml_b_f"][:L]
    rowrep = np.ascontiguousarray(np.broadcast_to(rowrep[:, None, :], (L, 128, NRR)))
    colpack = np.zeros((L, 128, NCP), np.float32)
    cw = np.concatenate([inp["ssd_conv_w"][:L], inp["ml_conv_w"][:L]], axis=2)
    cb = np.concatenate([inp["ssd_conv_b"][:L], inp["ml_conv_b"][:L]], axis=1)
    colpack[:, :, CP_CW:CP_CW + 40] = cw.reshape(L, 4, 10, 128).transpose(0, 3, 2, 1).reshape(L, 128, 40)
    colpack[:, :, CP_CB:CP_CB + 10] = cb.reshape(L, 10, 128).transpose(0, 2, 1)
    walaug = np.zeros((L, 33, 128), np.float32)
    walaug[:, 0:16, :] = inp["gla_w_alpha"][:L]
    walaug[:, 32, :] = inp["gla_b_alpha"][:L]
    fing = np.ascontiguousarray(np.broadcast_to(inp["final_norm_g"][None, :], (128, D)))
    return dict(wA=wA, rowrep=rowrep, colpack=colpack, walaug=walaug, fing=fing)


_CACHE = {}


def run(inputs, S, L, ncores):
    key = (S, L)
    if key not in _CACHE:
        nc = bass.Bass("TRN2", target_bir_lowering=False)
        Bld(nc, S, L).build()
        _CACHE[key] = nc
    nc = _CACHE[key]
    inp = {k: np.asarray(v) for k, v in inputs.items()}
    shared = dict(_consts(S))
    shared.update(_prep_weights(inp, L))
    ND = (L + 1) // 2
    NM = max(L // 2, 1)
    shared["w_in"] = inp["w_in"][:L]
    shared["w_branch"] = inp["w_branch"][:L]
    shared["w_out"] = inp["w_out"][:L]
    shared["ffn_w1"] = inp["ffn_w1"][:ND]
    shared["ffn_w3"] = inp["ffn_w3"][:ND]
    shared["ffn_w2"] = inp["ffn_w2"][:ND]
    shared["router_w"] = inp["router_w"][:NM]
    shared["moe_w1"] = inp["moe_w1"][:NM]
    shared["moe_w3"] = inp["moe_w3"][:NM]
    shared["moe_w2"] = inp["moe_w2"][:NM]
    shared = {k: np.ascontiguousarray(v) for k, v in shared.items()}
    maps = []
    for c in range(ncores):
        m = dict(shared)
        m["x"] = np.ascontiguousarray(inp["x"][c, :S])
        maps.append(m)
    res = run_bass_kernel_spmd(nc, maps, core_ids=list(range(ncores)))
    return np.stack([res.results[c]["out"] for c in range(ncores)], axis=0)


def kernel(**inputs):
    return run(inputs, 4096, 4, 8).astype(np.float32)
```
